# Optimizing a Trainium2 kernel written in Bass

```python
import jax, jax.numpy as jnp
from jax import lax
import numpy as np

D_MODEL = 1024
BATCH = 16
SEQ = 4096
DEPTH = 4

CHUNK = 64
NORM_EPS = 1e-6
A_WIDTH = D_MODEL // 2
A_EXPAND = 128
A_HEADS = A_WIDTH // A_EXPAND
A_DV = A_WIDTH // A_HEADS
B_HEADS = 4
B_DK = 64
B_DV = 2 * B_DK
B_QK = B_HEADS * B_DK
B_V = B_HEADS * B_DV
ROPE_BASE = 10000.0
C_HEADS = 4
C_DK = 64
C_DV = 128
C_QK = C_HEADS * C_DK
C_V = C_HEADS * C_DV
GK_RANK = 16
GK_NORMALIZER = 16.0
N_BRANCHES = 3
SPLIT_SIZES = (A_WIDTH, A_WIDTH, A_WIDTH, A_WIDTH,
               B_QK, B_QK, B_V, B_V,
               C_QK, C_QK, C_V, C_V, GK_RANK,
               N_BRANCHES * D_MODEL)
PROJ_WIDTH = int(sum(SPLIT_SIZES))
SPLIT_POINTS = tuple(int(s) for s in np.cumsum(SPLIT_SIZES)[:-1])
FFN_HIDDEN = ((8 * D_MODEL + 3 * 256 - 1) // (3 * 256)) * 256

kernel_name = "hybrid_hgrn2_retnet_gla_gated_trunk"


def rmsnorm(x, w, eps=NORM_EPS):
    xf = x.astype(jnp.float32)
    y = xf * lax.rsqrt(jnp.mean(xf * xf, axis=-1, keepdims=True) + eps)
    return (y * w.astype(jnp.float32)).astype(x.dtype)


def head_groupnorm(o, eps=NORM_EPS):
    of = o.astype(jnp.float32)
    mu = jnp.mean(of, axis=-1, keepdims=True)
    var = jnp.mean(jnp.square(of - mu), axis=-1, keepdims=True)
    return ((of - mu) * lax.rsqrt(var + eps)).astype(o.dtype)


def split_heads(t, n_heads):
    b, l, _ = t.shape
    return t.reshape(b, l, n_heads, -1).transpose(0, 2, 1, 3)


def merge_heads(t):
    b, h, l, d = t.shape
    return t.transpose(0, 2, 1, 3).reshape(b, l, h * d)


def rotary_every_two(t):
    seq, dk = t.shape[2], t.shape[3]
    inv_freq = 1.0 / (ROPE_BASE ** jnp.linspace(0.0, 1.0, dk // 2, dtype=jnp.float32))
    ang = jnp.arange(seq, dtype=jnp.float32)[:, None] * inv_freq[None, :]
    sin, cos = jnp.sin(ang), jnp.cos(ang)
    tf = t.astype(jnp.float32)
    t1, t2 = tf[..., 0::2], tf[..., 1::2]
    out = jnp.stack([t1 * cos - t2 * sin, t1 * sin + t2 * cos], axis=-1).reshape(t.shape)
    return out.astype(t.dtype)


def chunked_gated_linear_attention(q, k, v, log_decay):
    bsz, nh, seq, dk = q.shape
    dv = v.shape[-1]
    dg = log_decay.shape[-1]
    n_chunks = seq // CHUNK

    def to_chunks(t):
        return t.reshape(bsz, nh, n_chunks, CHUNK, t.shape[-1]).transpose(2, 0, 1, 3, 4)

    qc, kc, vc = to_chunks(q), to_chunks(k), to_chunks(v)
    gc = to_chunks(log_decay.astype(jnp.float32))
    causal = jnp.tril(jnp.ones((CHUNK, CHUNK), dtype=bool))[:, :, None]

    def step(state, chunk):
        qi, ki, vi, gi = chunk
        b = jnp.cumsum(gi, axis=2)
        b_last = b[:, :, -1:, :]
        o_inter = jnp.einsum('bhcd,bhdv->bhcv', qi * jnp.exp(b), state)
        rel = jnp.where(causal, b[:, :, :, None, :] - b[:, :, None, :, :], -jnp.inf)
        if dg == 1:
            scores = jnp.einsum('bhid,bhjd->bhij', qi, ki) * jnp.exp(rel[..., 0])
        else:
            scores = jnp.sum(qi[:, :, :, None, :] * ki[:, :, None, :, :] * jnp.exp(rel), axis=-1)
        o = o_inter + jnp.einsum('bhij,bhjv->bhiv', scores, vi)
        state = state * jnp.swapaxes(jnp.exp(b_last), -1, -2) + jnp.einsum(
            'bhcd,bhcv->bhdv', ki * jnp.exp(b_last - b), vi)
        return state, o

    state0 = jnp.zeros((bsz, nh, dk, dv), jnp.float32)
    _, oc = lax.scan(step, state0, (qc, kc, vc, gc))
    return oc.transpose(1, 2, 0, 3, 4).reshape(bsz, nh, seq, dv).astype(v.dtype)


def hybrid_mixer(xn, w_in, lb, w_gk_up, b_gk, gn_a, gn_c, w_br_a, w_br_b, w_br_c, w_out):
    bsz, seq, _ = xn.shape
    proj = jnp.einsum('bld,dp->blp', xn, w_in)
    (a_q, a_f, a_i, a_g, b_q, b_k, b_v, b_g,
     c_q, c_k, c_v, c_g, c_r, gate_logits) = jnp.split(proj, SPLIT_POINTS, axis=-1)

    zf = a_f.astype(jnp.float32)
    lbf = lb.astype(jnp.float32)
    log_f = jnp.logaddexp(jnp.log(lbf), jnp.log1p(-lbf) + jax.nn.log_sigmoid(zf))
    k_a = ((1.0 - lbf) * jax.nn.sigmoid(-zf)).astype(xn.dtype)
    q_a = split_heads(jax.nn.silu(a_q), A_HEADS) * (A_EXPAND ** -0.5)
    o_a = chunked_gated_linear_attention(q_a, split_heads(k_a, A_HEADS), split_heads(a_i, A_HEADS),
                                         split_heads(log_f, A_HEADS))
    o_a = merge_heads(rmsnorm(o_a, gn_a)) * jax.nn.silu(a_g)

    q_b = rotary_every_two(split_heads(b_q, B_HEADS))
    k_b = rotary_every_two(split_heads(b_k, B_HEADS)) * (B_DK ** -0.5)
    log_gamma = jnp.log(1.0 - 2.0 ** (-5.0 - jnp.arange(B_HEADS, dtype=jnp.float32)))
    g_b = jnp.broadcast_to(log_gamma[None, :, None, None], (bsz, B_HEADS, seq, 1))
    o_b = chunked_gated_linear_attention(q_b, k_b, split_heads(b_v, B_HEADS), g_b)
    o_b = merge_heads(head_groupnorm(o_b)) * jax.nn.silu(b_g)

    gk_logits = jnp.einsum('blr,rk->blk', c_r, w_gk_up) + b_gk
    log_gk = jax.nn.log_sigmoid(gk_logits.astype(jnp.float32)) / GK_NORMALIZER
    q_c = split_heads(c_q, C_HEADS) * (C_DK ** -0.5)
    o_c = chunked_gated_linear_attention(q_c, split_heads(c_k, C_HEADS), split_heads(c_v, C_HEADS),
                                         split_heads(log_gk, C_HEADS))
    o_c = merge_heads(rmsnorm(o_c, gn_c)) * jax.nn.silu(c_g)

    gates = jax.nn.sigmoid(gate_logits).reshape(bsz, seq, N_BRANCHES, D_MODEL)
    merged = (gates[:, :, 0] * jnp.einsum('blc,cd->bld', o_a, w_br_a)
              + gates[:, :, 1] * jnp.einsum('blc,cd->bld', o_b, w_br_b)
              + gates[:, :, 2] * jnp.einsum('blc,cd->bld', o_c, w_br_c))
    return jnp.einsum('bld,de->ble', merged, w_out)


def swiglu_ffn(xn, w_gate, w_up, w_down):
    h = jax.nn.silu(jnp.einsum('bld,df->blf', xn, w_gate)) * jnp.einsum('bld,df->blf', xn, w_up)
    return jnp.einsum('blf,fd->bld', h, w_down)


def setup_inputs(seed: int = 0) -> dict:
    key = jax.random.key(seed)
    ks = jax.random.split(key, 17)
    f32 = jnp.float32
    nrm = jax.random.normal
    return {
        "x": nrm(ks[0], (BATCH, SEQ, D_MODEL), f32),
        "norm_mix": 1.0 + 0.02 * nrm(ks[1], (DEPTH, D_MODEL), f32),
        "w_in": nrm(ks[2], (DEPTH, D_MODEL, PROJ_WIDTH), f32) * D_MODEL ** -0.5,
        "lb_logits": 0.1 * nrm(ks[3], (DEPTH, A_WIDTH), f32),
        "w_gk_up": nrm(ks[4], (DEPTH, GK_RANK, C_QK), f32) * GK_RANK ** -0.5,
        "b_gk": 0.1 * nrm(ks[5], (DEPTH, C_QK), f32),
        "gn_a": 1.0 + 0.02 * nrm(ks[6], (DEPTH, A_DV), f32),
        "gn_c": 1.0 + 0.02 * nrm(ks[7], (DEPTH, C_DV), f32),
        "w_br_a": nrm(ks[8], (DEPTH, A_WIDTH, D_MODEL), f32) * A_WIDTH ** -0.5,
        "w_br_b": nrm(ks[9], (DEPTH, B_V, D_MODEL), f32) * B_V ** -0.5,
        "w_br_c": nrm(ks[10], (DEPTH, C_V, D_MODEL), f32) * C_V ** -0.5,
        "w_out": nrm(ks[11], (DEPTH, D_MODEL, D_MODEL), f32) * D_MODEL ** -0.5,
        "norm_ffn": 1.0 + 0.02 * nrm(ks[12], (DEPTH, D_MODEL), f32),
        "w_ffn_gate": nrm(ks[13], (DEPTH, D_MODEL, FFN_HIDDEN), f32) * D_MODEL ** -0.5,
        "w_ffn_up": nrm(ks[14], (DEPTH, D_MODEL, FFN_HIDDEN), f32) * D_MODEL ** -0.5,
        "w_ffn_down": nrm(ks[15], (DEPTH, FFN_HIDDEN, D_MODEL), f32) * FFN_HIDDEN ** -0.5,
        "norm_final": 1.0 + 0.02 * nrm(ks[16], (D_MODEL,), f32),
    }


def reference(x, norm_mix, w_in, lb_logits, w_gk_up, b_gk, gn_a, gn_c, w_br_a, w_br_b, w_br_c,
              w_out, norm_ffn, w_ffn_gate, w_ffn_up, w_ffn_down, norm_final):
    lb_all = jnp.cumsum(jax.nn.softmax(lb_logits.astype(jnp.float32), axis=0), axis=0)
    lb_all = lb_all - lb_all[0:1]
    for layer in range(DEPTH):
        h = x + hybrid_mixer(rmsnorm(x, norm_mix[layer]), w_in[layer], lb_all[layer], w_gk_up[layer],
                             b_gk[layer], gn_a[layer], gn_c[layer], w_br_a[layer], w_br_b[layer],
                             w_br_c[layer], w_out[layer])
        x = h + swiglu_ffn(rmsnorm(h, norm_ffn[layer]), w_ffn_gate[layer], w_ffn_up[layer],
                           w_ffn_down[layer])
    return rmsnorm(x, norm_final)
```

```python
import numpy as np
from contextlib import ExitStack
import concourse.bass as bass
import concourse.mybir as mybir
from concourse.bass_utils import run_bass_kernel_spmd

F32 = mybir.dt.float32
BF = mybir.dt.bfloat16
AF = mybir.ActivationFunctionType
ALU = mybir.AluOpType
AX = mybir.AxisListType

D = 1024
DEPTH = 4
SEQ = 4096
BATCH = 16
NCORES = 8
_TEST_CORES = 0
T = 512
NSUB = 4
KC = 8
FFN = 2816
FC = FFN // 128
PROJ = 8208
EPS = 1e-6
NBUF = 3
NSCR = 5
SLOTW = 4096
CLAMP = 40.0

C_AQ, C_AF, C_AI, C_AG = 0, 512, 1024, 1536
C_BQ, C_BK, C_BV, C_BG = 2048, 2304, 2560, 3072
C_CQ, C_CK, C_CV, C_CG, C_CR = 3584, 3840, 4096, 4608, 5120
C_MG = 5136


def _swap_cols(c0, n):
    idx = np.arange(n)
    return c0 + (idx ^ 1)


def fm_chunk_list():
    L = [("c_r", 0, np.arange(C_CR, C_CR + 16))]
    for c in range(4):
        L.append(("a_f", c, np.arange(C_AF + 128 * c, C_AF + 128 * (c + 1))))
        L.append(("a_q", c, np.arange(C_AQ + 128 * c, C_AQ + 128 * (c + 1))))
    for c in range(2):
        L.append(("c_q", c, np.arange(C_CQ + 128 * c, C_CQ + 128 * (c + 1))))
        L.append(("c_k", c, np.arange(C_CK + 128 * c, C_CK + 128 * (c + 1))))
    for c in range(2):
        L.append(("b_q", c, np.arange(C_BQ + 128 * c, C_BQ + 128 * (c + 1))))
        L.append(("b_qs", c, _swap_cols(C_BQ + 128 * c, 128)))
        L.append(("b_k", c, np.arange(C_BK + 128 * c, C_BK + 128 * (c + 1))))
        L.append(("b_ks", c, _swap_cols(C_BK + 128 * c, 128)))
    for g in range(24):
        L.append(("mg", g, np.arange(C_MG + 128 * g, C_MG + 128 * (g + 1))))
    return L


FM = fm_chunk_list()
NFM_SLOTS = (len(FM) + 3) // 4
TOK_COLS = [C_AI, C_AG, C_BV, C_BG, C_CV, C_CG]
S_TOK = NFM_SLOTS
S_BR = S_TOK + 6
S_OUT = S_BR + 3
S_FFN = S_OUT + 2
S_DOWN = S_FFN + 11
NSLOT = S_DOWN + 8


def slot_width(slot):
    if slot < NFM_SLOTS:
        n = min(4, len(FM) - 4 * slot)
        return n * 1024
    if slot >= S_DOWN:
        return FC * 128
    return SLOTW


def const_layout():
    off = {}
    cur = [0]

    def add(name, n):
        off[name] = cur[0]
        cur[0] += n

    add("nmix", DEPTH * 8)
    add("nffn", DEPTH * 8)
    add("nfin", 8)
    add("lbl", DEPTH * 4)
    add("bgk", DEPTH * 2)
    add("gna", DEPTH)
    add("gnc", DEPTH)
    add("ident", 128)
    add("mask", 128)
    add("cmask", 512)
    add("eqBz", 4 * 64)
    add("ekB", 2 * 64)
    add("erefB", 2)
    add("elastB", 2)
    add("chm", 2)
    add("chmc2B", 4)
    add("hm8", 2)
    add("eps", 1)
    return off, cur[0]


COFF, NCONST = const_layout()


def build_consts(inp):
    c = np.zeros((128, NCONST), np.float32)
    p = np.arange(128)

    def put(name, arr):
        arr = np.asarray(arr, np.float32)
        c[:, COFF[name]:COFF[name] + arr.shape[1]] = arr

    def fm(v):
        L = v.shape[0]
        n = v.shape[1] // 128
        return np.ascontiguousarray(v.reshape(L, n, 128).transpose(2, 0, 1).reshape(128, L * n))

    put("nmix", fm(inp["norm_mix"]))
    put("nffn", fm(inp["norm_ffn"]))
    put("nfin", fm(inp["norm_final"][None, :]))
    put("lbl", fm(inp["lb_logits"]))
    put("bgk", fm(inp["b_gk"]))
    put("gna", fm(inp["gn_a"]))
    put("gnc", fm(inp["gn_c"]))
    put("ident", np.eye(128, dtype=np.float32))
    j = p[:, None]
    i = p[None, :]
    put("mask", ((j <= i) & ((j // 64) == (i // 64))).astype(np.float32))
    t = np.arange(512)
    put("cmask", np.broadcast_to((t % 64 != 0).astype(np.float32)[None, :], (128, 512)))
    lg = np.log(1.0 - 2.0 ** (-5.0 - np.arange(4, dtype=np.float64)))
    tt = np.arange(64, dtype=np.float64)
    eqBz = np.zeros((128, 4 * 64))
    ekB = np.zeros((128, 2 * 64))
    erefB = np.zeros((128, 2))
    elastB = np.zeros((128, 2))
    c2B = np.zeros((128, 2))
    for h in range(4):
        cch, half = h // 2, h % 2
        rows = slice(64 * half, 64 * half + 64)
        eqBz[rows, h * 64:(h + 1) * 64] = np.exp(lg[h] * (tt - 32.0))[None, :]
        ekB[rows, cch * 64:(cch + 1) * 64] = (np.exp(-lg[h] * (tt - 32.0)) * 0.125)[None, :]
        erefB[rows, cch] = np.exp(lg[h] * 33.0)
        elastB[rows, cch] = np.exp(lg[h] * 64.0)
        c2B[rows, cch] = np.exp(lg[h] * 31.0)
    put("eqBz", eqBz)
    put("ekB", ekB)
    put("erefB", erefB)
    put("elastB", elastB)
    chm = np.zeros((128, 2))
    chm[:64, 0] = 1.0
    chm[64:, 1] = 1.0
    put("chm", chm)
    chmc2B = np.zeros((128, 4))
    for cch in range(2):
        for ch in range(2):
            chmc2B[:, cch * 2 + ch] = chm[:, ch] * 1.0
    put("chmc2B", chmc2B)
    hm8 = np.zeros((128, 2))
    hm8[:64, 0] = 0.125
    hm8[64:, 1] = 0.125
    put("hm8", hm8)
    wgk = np.ascontiguousarray(inp["w_gk_up"].transpose(1, 0, 2).reshape(16, DEPTH * 256), dtype=np.float32)
    put("eps", np.full((128, 1), EPS, np.float32))
    extra = {"c2B": c2B.astype(np.float32), "wgk": wgk}
    return c, extra


def rope_tables(seq):
    inv_freq = (1.0 / (10000.0 ** np.linspace(0.0, 1.0, 32, dtype=np.float32))).astype(np.float32)
    ang = np.arange(seq, dtype=np.float32)[:, None] * inv_freq[None, :]
    sin, cos = np.sin(ang), np.cos(ang)
    f = np.arange(128) % 64
    cosT = cos[:, f // 2].T.astype(np.float32)
    sgn = np.where(f % 2 == 0, -1.0, 1.0).astype(np.float32)
    sinT = (sin[:, f // 2].T * sgn[:, None]).astype(np.float32)
    return np.ascontiguousarray(cosT), np.ascontiguousarray(sinT)


def build_wsrc(inp, depth):
    w = np.zeros((depth, NSLOT, 128, SLOTW), np.float32)

    def kmaj(m):
        K = m.shape[0] // 128
        return m.reshape(K, 128, m.shape[1]).transpose(1, 0, 2)

    for l in range(depth):
        win = inp["w_in"][l]
        for ci, (kind, idx, cols) in enumerate(FM):
            slot, pos = ci // 4, ci % 4
            blk = kmaj(win[:, cols])
            w[l, slot].reshape(128, 4, 8, 128)[:, pos, :, :blk.shape[2]] = blk
        for t_, c0 in enumerate(TOK_COLS):
            w[l, S_TOK + t_] = kmaj(win[:, c0:c0 + 512]).reshape(128, SLOTW)
        for br, nm in enumerate(("w_br_a", "w_br_b", "w_br_c")):
            wb = kmaj(inp[nm][l])
            dst = w[l, S_BR + br].reshape(128, 8, 4, 128)
            dst[:] = wb.reshape(128, 4, 8, 128).transpose(0, 2, 1, 3)
        wo = kmaj(inp["w_out"][l])
        for i in range(2):
            dst = w[l, S_OUT + i].reshape(128, 4, 8, 128)
            dst[:] = wo[:, :, i * 512:(i + 1) * 512].reshape(128, 8, 4, 128).transpose(0, 2, 1, 3)
        wg = kmaj(inp["w_ffn_gate"][l])
        wu = kmaj(inp["w_ffn_up"][l])
        for i in range(11):
            dst = w[l, S_FFN + i].reshape(128, 4, 8, 128)
            for j in range(2):
                fc = 2 * i + j
                dst[:, 2 * j] = wg[:, :, fc * 128:(fc + 1) * 128]
                dst[:, 2 * j + 1] = wu[:, :, fc * 128:(fc + 1) * 128]
        wd = kmaj(inp["w_ffn_down"][l])
        for dc in range(8):
            w[l, S_DOWN + dc, :, :FC * 128] = wd[:, :, dc * 128:(dc + 1) * 128].reshape(128, FC * 128)
    return w


ENGS = ("pe", "act", "dve", "pool", "sp")


class Op:
    __slots__ = ("eng", "fn", "deps", "needed", "cnt", "sem", "dma")

    def __init__(self, eng, fn, dma):
        self.eng = eng
        self.fn = fn
        self.deps = ()
        self.needed = False
        self.cnt = 0
        self.sem = None
        self.dma = dma


class Prog:
    def __init__(self):
        self.q = {e: [] for e in ENGS}
        self.last_w = {}
        self.readers = {}
        self.dma_keys = {}

    def op(self, eng, fn, reads=(), writes=(), dma_key=None):
        o = Op(eng, fn, dma_key)
        deps = set()
        lw, rd = self.last_w, self.readers
        for r in reads:
            w = lw.get(r)
            if w is not None:
                deps.add(w)
        for r in writes:
            w = lw.get(r)
            if w is not None:
                deps.add(w)
            rs = rd.get(r)
            if rs:
                deps.update(rs)
        for r in reads:
            rd.setdefault(r, []).append(o)
        for r in writes:
            lw[r] = o
            rd[r] = []
        o.deps = tuple(deps)
        for d in deps:
            d.needed = True
        self.q[eng].append(o)
        if dma_key is not None:
            self.dma_keys[dma_key] = None
        return o

    def finalize(self, getsem):
        self.eng_sem = {e: getsem("c_" + e) for e in ENGS}
        self.dma_sem = {k: getsem("d_%d" % i) for i, k in enumerate(self.dma_keys)}
        self.dma_cnt = {k: 0 for k in self.dma_keys}
        for e in ENGS:
            c = 0
            for o in self.q[e]:
                if o.dma is not None:
                    self.dma_cnt[o.dma] += 16
                    o.sem = self.dma_sem[o.dma]
                    o.cnt = self.dma_cnt[o.dma]
                elif o.needed:
                    c += 1
                    o.sem = self.eng_sem[e]
                    o.cnt = c

    def emit_engine(self, e, engobj, final_waits=False):
        waited = {}
        for o in self.q[e]:
            need = {}
            for d in o.deps:
                if e == "pe" and d.eng == "pe" and d.dma is None:
                    continue
                s = d.sem
                k = id(s)
                if need.get(k, (None, 0))[1] < d.cnt:
                    need[k] = (s, d.cnt)
            for k, (s, c) in need.items():
                if waited.get(k, 0) >= c:
                    continue
                engobj.wait_ge(s, c)
                waited[k] = c
            ins = o.fn(engobj)
            if o.dma is not None:
                ins.then_inc(o.sem, 16)
            elif o.needed:
                ins.then_inc(o.sem, 1)
        if final_waits:
            for k, s in self.dma_sem.items():
                if self.dma_cnt[k] > 0:
                    engobj.wait_ge(s, self.dma_cnt[k])


def build_program(n_seq, n_tiles, depth, debug=False):
    seq_t = n_tiles * T
    ntok = n_seq * seq_t
    nc = bass.Bass("TRN2", target_bir_lowering=False)
    x_d = nc.dram_tensor("x", [ntok, D], F32, kind="ExternalInput").ap()
    wsrc_d = nc.dram_tensor("wsrc", [depth, NSLOT, 128, SLOTW], F32, kind="ExternalInput").ap()
    cst_d = nc.dram_tensor("cst", [128, NCONST], F32, kind="ExternalInput").ap()
    c2b_d = nc.dram_tensor("c2b", [128, 2], F32, kind="ExternalInput").ap()
    wgk_d = nc.dram_tensor("wgk", [16, DEPTH * 256], F32, kind="ExternalInput").ap()
    cos_d = nc.dram_tensor("cosT", [128, seq_t], F32, kind="ExternalInput").ap()
    sin_d = nc.dram_tensor("sinT", [128, seq_t], F32, kind="ExternalInput").ap()
    out_d = nc.dram_tensor("out", [ntok, D], F32, kind="ExternalOutput").ap()
    wst_d = nc.dram_tensor("wst", [depth, NSLOT, 128, SLOTW], BF, kind="Internal").ap()

    P = Prog()
    es = ExitStack()
    with es:
        def sb(name, shape, dt):
            return es.enter_context(nc.sbuf_tensor("s_" + name, shape, dt))

        def ps(name, shape, dt):
            return es.enter_context(nc.psum_tensor(name, shape, dt))

        xT = sb("xT", [128, 8, T], F32)
        xn = sb("xn", [128, 8, T], BF)
        rstd = sb("rstd", [128, T], F32)
        big = sb("big", [128, 24 * 512], BF)
        tokv = sb("tokv", [128, NSUB, 3072], BF)
        ring = sb("ring", [128, NBUF, SLOTW], BF)
        QT = sb("QT", [128, 12, T], BF)
        KT = sb("KT", [128, 8, T], BF)
        K2T = sb("K2T", [128, 3, T], BF)
        K2tok = sb("K2tok", [128, 2, NSUB, 1024], BF)
        scm = sb("scm", [128, 2, 512], BF)
        ybuf = sb("ybuf", [128, 2, 1536], BF)
        yT = sb("yT", [128, 12, T], BF)
        S = sb("S", [128, depth, 8, 128], F32)
        sref = sb("sref", [128, 2, 8, 128], BF)
        eref = sb("eref", [128, 8, 8], F32)
        elast = sb("elast", [128, 8, 8], F32)
        scr = sb("scr", [128, NSCR, T], F32)
        stt_ = sb("stt", [128, 8, 8], F32)
        cst = sb("cst", [128, NCONST], F32)
        c2b = sb("c2b", [128, 2], F32)
        cosb = sb("cosb", [128, T], F32)
        sinb = sb("sinb", [128, T], F32)
        crT = sb("crT", [16, T], BF)
        identb = sb("identb", [128, 128], BF)
        onesb = sb("onesb", [128, 128], BF)
        mask4 = sb("mask4", [128, 4, 128], BF)
        wgkb = sb("wgkb", [16, depth * 256], BF)
        lbt = sb("lbt", [128, 3, depth * 4], F32)
        lbw = sb("lbw", [128, 4, 8], F32)
        negbgk = sb("negbgk", [128, depth * 2], F32)

        mm = [ps("mm%d" % i, [128, 512], F32) for i in range(3)]
        p_sc = ps("p_sc", [128, 512], F32)
        p_o = [ps("p_o%d" % i, [128, 512], F32) for i in range(2)]
        p_kv = ps("p_kv", [128, 512], F32)
        p_tr = ps("p_tr", [128, 2, 512], BF)

        sems = {}

        def getsem(name):
            if name not in sems:
                sems[name] = es.enter_context(nc.semaphore(name))
            return sems[name]

        def dma(eng, out, in_, key, reads, writes):
            P.op(eng, lambda e: e.dma_start(out=out, in_=in_), reads, writes, dma_key=key)

        def act(out, in_, func, reads, writes, bias=None, scale=None):
            kw = {}
            if bias is not None:
                kw["bias"] = bias
            if scale is not None:
                kw["scale"] = scale
            P.op("act", lambda e: e.activation(out=out, in_=in_, func=func, **kw), reads, writes)

        def tt(eng, out, in0, in1, op, reads, writes):
            P.op(eng, lambda e: e.tensor_tensor(out=out, in0=in0, in1=in1, op=op), reads, writes)

        def ts(eng, out, in0, s1, s2, op0, op1, reads, writes):
            if op1 is None:
                P.op(eng, lambda e: e.tensor_scalar(out=out, in0=in0, scalar1=s1, scalar2=None, op0=op0),
                     reads, writes)
            else:
                P.op(eng, lambda e: e.tensor_scalar(out=out, in0=in0, scalar1=s1, scalar2=s2, op0=op0, op1=op1),
                     reads, writes)

        def stt(eng, out, in0, scalar, in1, op0, op1, reads, writes):
            P.op(eng, lambda e: e.scalar_tensor_tensor(out=out, in0=in0, scalar=scalar, in1=in1, op0=op0, op1=op1),
                 reads, writes)

        def cp(eng, out, in_, reads, writes):
            if eng == "act":
                act(out, in_, AF.Copy, reads, writes)
            else:
                P.op(eng, lambda e: e.tensor_copy(out=out, in_=in_), reads, writes)

        def mmg(out, pairs, reads, writes):
            n = len(pairs)

            def fn(e):
                ins = None
                for i, (l_, r_) in enumerate(pairs):
                    ins = e.matmul(out, l_, r_, start=(i == 0), stop=(i == n - 1))
                return ins
            P.op("pe", fn, reads, writes)

        def pe_multi(items, reads, writes):
            def fn(e):
                ins = None
                for (kind, o_, a_, b_, st, sp_) in items:
                    if kind == "mm":
                        ins = e.matmul(o_, a_, b_, start=st, stop=sp_)
                    else:
                        ins = e.transpose(o_, a_, b_)
                return ins
            P.op("pe", fn, reads, writes)

        rot = {"mm": 0, "scr": 0, "o": 0, "scm": 0, "k2t": 0, "tr": 0, "st": 0, "w": 0, "cast": 0}

        def nxt(name, n):
            v = rot[name]
            rot[name] = (v + 1) % n
            return v

        MM_FULL = [(mm[0], ("mm", 0)), (mm[1], ("mm", 1)), (mm[2], ("mm", 2)), (p_sc, ("sc",)),
                   (p_o[0], ("o", 0)), (p_o[1], ("o", 1)), (p_kv, ("kv",))]

        def mmbank():
            i = nxt("mm", len(MM_FULL))
            return MM_FULL[i]

        def scratch():
            i = nxt("scr", NSCR)
            return scr[:, i, :], ("scr", i)

        def stat():
            i = nxt("st", 8)
            return stt_[:, i, :], ("st", i)

        BIG_ALL = [("big", u) for u in range(24)]
        TOKV_ALL = [("tokv", s, g) for s in range(NSUB) for g in range(6)]
        CST = ("cst",)

        def cc(name, i=0, n=1):
            o = COFF[name] + i
            return cst[:, o:o + n]

        dma("sp", cst[:], cst_d, "cst", [], [CST])
        dma("sp", c2b[:], c2b_d, "c2b", [], [("c2b",)])
        cp("dve", identb[:], cc("ident", 0, 128), [CST], [("identb",)])
        P.op("dve", lambda e: e.memset(onesb[:], 1.0), [], [("onesb",)])
        for h in range(4):
            cp("dve", mask4[:, h, :], cc("mask", 0, 128), [CST], [("mask4",)])
        dma("pool", wgkb[:], wgk_d[:, 0:depth * 256], "wgkb", [], [("wgkb",)])
        ts("dve", negbgk[:], cc("bgk", 0, depth * 2), -1.0, None, ALU.mult, None, [CST], [("negbgk",)])
        cp("dve", eref[:, 4:6, :], cc("erefB", 0, 2).unsqueeze(2).to_broadcast([128, 2, 8]), [CST],
           [("eref", 4), ("eref", 5)])
        cp("dve", elast[:, 4:6, :], cc("elastB", 0, 2).unsqueeze(2).to_broadcast([128, 2, 8]), [CST],
           [("elast", 4), ("elast", 5)])
        lbl3 = cst[:, COFF["lbl"]:COFF["lbl"] + 16].rearrange("p (l f) -> p l f", l=4)
        act(lbw[:, :, 0:4], lbl3, AF.Exp, [CST], [("lbw",)])
        P.op("dve", lambda e: e.tensor_reduce(out=lbw[:, 0, 4:8], in_=lbw[:, :, 0:4].rearrange("p l f -> p f l"),
                                              axis=AX.X, op=ALU.add), [("lbw",)], [("lbw2",)])
        P.op("dve", lambda e: e.reciprocal(out=lbw[:, 1, 4:8], in_=lbw[:, 0, 4:8]), [("lbw2",)], [("lbw3",)])
        tt("dve", lbw[:, :, 0:4], lbw[:, :, 0:4], lbw[:, 1:2, 4:8].to_broadcast([128, 4, 4]), ALU.mult,
           [("lbw",), ("lbw3",)], [("lbw",)])
        LBK = ("lbt",)
        P.op("dve", lambda e: e.memset(lbt[:, 0, 0:4], 0.0), [], [LBK])
        for l in range(1, depth):
            tt("dve", lbt[:, 0, 4 * l:4 * l + 4], lbt[:, 0, 4 * l - 4:4 * l], lbw[:, l, 0:4], ALU.add,
               [LBK, ("lbw",)], [LBK])
        ts("dve", lbt[:, 1, :], lbt[:, 0, :], -1.0, 1.0, ALU.mult, ALU.add, [LBK], [("lbt1",)])
        ts("dve", lbt[:, 2, :], lbt[:, 0, :], 1.0, -1.0, ALU.mult, ALU.add, [LBK], [("lbt2",)])
        LBR = [LBK, ("lbt1",), ("lbt2",)]

        st32 = [tokv[:].rearrange("p s c -> p (s c)")[:, 0:2 * SLOTW].bitcast(F32),
                big[:, 0:2 * SLOTW].bitcast(F32),
                xT[:].rearrange("p a b -> p (a b)")]
        st32_keys = [TOKV_ALL, BIG_ALL, [("xT", dc) for dc in range(8)]]
        cast_engs = ["dve", "act"]
        k = 0
        for l in range(depth):
            for slot in range(NSLOT):
                w_ = slot_width(slot)
                i = k % 3
                dma("sp", st32[i][:, :w_], wsrc_d[l, slot, :, :w_], ("st32", i), [], st32_keys[i])
                ce = cast_engs[k % 2]
                cp(ce, ring[:, i, :w_], st32[i][:, :w_], st32_keys[i], [("wr", i)])
                dma("sp", wst_d[l, slot, :, :w_], ring[:, i, :w_], ("st16", i), [("wr", i)], [("wst", l, slot)])
                k += 1

        wcount = [0]

        def use_slot(l, slot):
            ri = wcount[0] % NBUF
            wcount[0] += 1
            w_ = slot_width(slot)
            dma("sp", ring[:, ri, :w_], wst_d[l, slot, :, :w_], ("wr", ri), [("wst", l, slot)], [("wr", ri)])
            return ri

        XT_ALL = [("xT", dc) for dc in range(8)]
        XN_ALL = [("xn", dc) for dc in range(8)]

        def rmsnorm_to_xn(wname, wl):
            for dc in range(8):
                if dc < 5:
                    act(xn[:, dc, :], xT[:, dc, :], AF.Square, [("xT", dc)], [("xn", dc)])
                else:
                    tt("pool", xn[:, dc, :], xT[:, dc, :], xT[:, dc, :], ALU.mult, [("xT", dc)], [("xn", dc)])
            bank, bk = mmbank()
            mmg(bank[:, :], [(onesb[:], xn[:, dc, :]) for dc in range(8)], XN_ALL + [("onesb",)], [bk])
            act(rstd[:], bank[:, :], AF.Ln, [bk, CST], [("rstd",)], scale=1.0 / D, bias=cc("eps", 0, 1))
            act(rstd[:], rstd[:], AF.Exp, [("rstd",)], [("rstd",)], scale=-0.5)
            for dc in range(8):
                eng = "dve"
                stt(eng, xn[:, dc, :], xT[:, dc, :], cc(wname, wl * 8 + dc, 1), rstd[:], ALU.mult, ALU.mult,
                    [("xT", dc), ("rstd",), CST], [("xn", dc)])

        def gate_generic(Bt, Bk, blk, gs):
            B3 = Bt.rearrange("p (c t) -> p c t", t=64)
            act(eref[:, blk, :], B3[:, :, 32], AF.Exp, [Bk], [("eref", blk)], scale=gs)
            act(elast[:, blk, :], B3[:, :, 63], AF.Exp, [Bk], [("elast", blk)], scale=gs)
            Bc, Bck = scratch()
            Bc3 = Bc.rearrange("p (c t) -> p c t", t=64)
            tt("dve", Bc3, B3, B3[:, :, 32:33].to_broadcast([128, 8, 64]), ALU.subtract, [Bk], [Bck])
            lim = CLAMP / abs(gs)
            ts("dve", Bc, Bc, -lim, lim, ALU.max, ALU.min, [Bck], [Bck])
            eq, eqk = scratch()
            ek, ekk = scratch()
            act(eq, Bc, AF.Exp, [Bck], [eqk], scale=gs)
            act(ek, Bc, AF.Exp, [Bck], [ekk], scale=-gs)
            return eq, eqk, ek, ekk

        k2_pend = []

        def k2_transposes(src, srck, blk, scale_cols):
            k2_pend.append((src, srck, blk, scale_cols))

        def k2_flush(keep):
            while len(k2_pend) > keep:
                k2_transposes_now(*k2_pend.pop(0))

        def k2_transposes_now(src, srck, blk, scale_cols):
            half = nxt("tr", 2)
            items = []
            for s in range(NSUB):
                items.append(("tr", p_tr[:, half, s * 128:(s + 1) * 128], src[:, s * 128:(s + 1) * 128], identb[:],
                              True, True))
            pe_multi(items, [srck, ("identb",)], [("tr",)])
            for ch in range(2):
                act(K2tok[:, ch, :, blk * 128:(blk + 1) * 128],
                    p_tr[:, half, :].rearrange("p (s d) -> p s d", s=NSUB), AF.Copy,
                    [("tr",), CST], [("K2tok", ch, blk)], scale=scale_cols[ch])

        chm_cols = [cc("chm", 0, 1), cc("chm", 1, 1)]

        def layer(l, tile_idx):
            rmsnorm_to_xn("nmix", l)
            ring_of = {}
            state = {}
            for ci, (kind, idx, cols) in enumerate(FM):
                slot, pos = ci // 4, ci % 4
                if pos == 0:
                    ring_of[slot] = use_slot(l, slot)
                ri = ring_of[slot]
                M = len(cols)
                bank, bk = mmbank()

                def grp(bank=bank, bk=bk, ri=ri, pos=pos, M=M):
                    mmg(bank[0:M, :], [(ring[:, ri, (pos * 8 + kc) * 128:(pos * 8 + kc) * 128 + M], xn[:, kc, :])
                                       for kc in range(8)], XN_ALL + [("wr", ri)], [bk])
                grp()
                if kind == "c_r":
                    cp("act", crT[:, :], bank[0:16, :], [bk], [("crT",)])
                elif kind == "a_f":
                    c = idx
                    sig, sigk = scratch()
                    act(sig, bank[:, :], AF.Sigmoid, [bk], [sigk])
                    lf, lfk = scratch()
                    act(lf, sig, AF.Ln, [sigk] + LBR, [lfk], scale=lbt[:, 1, 4 * l + c:4 * l + c + 1],
                        bias=lbt[:, 0, 4 * l + c:4 * l + c + 1])
                    ka, kak = scratch()
                    ts("dve", ka, sig, lbt[:, 2, 4 * l + c:4 * l + c + 1], lbt[:, 1, 4 * l + c:4 * l + c + 1],
                       ALU.mult, ALU.add, [sigk] + LBR, [kak])
                    Bt, Bk = scratch()
                    P.op("dve", lambda e, Bt=Bt, lf=lf: e.tensor_tensor_scan(
                        out=Bt, data0=cc("cmask", 0, 512), data1=lf, initial=0.0, op0=ALU.mult, op1=ALU.add),
                        [lfk, CST], [Bk])
                    eq, eqk, ek, ekk = gate_generic(Bt, Bk, c, 1.0)
                    tt("pool", KT[:, c, :], ka, ek, ALU.mult, [kak, ekk], [("KT", c)])
                    k2_flush(2)
                    j = nxt("k2t", 3)
                    tt("pool", K2T[:, j, :].rearrange("p (c t) -> p c t", t=64),
                       KT[:, c, :].rearrange("p (c t) -> p c t", t=64),
                       eq.rearrange("p (c t) -> p c t", t=64)[:, :, 63:64].to_broadcast([128, 8, 64]), ALU.mult,
                       [("KT", c), eqk], [("K2T", j)])
                    k2_transposes(K2T[:, j, :], ("K2T", j), c, chm_cols)
                    state["eqA"] = (eq, eqk)
                elif kind == "a_q":
                    c = idx
                    eq, eqk = state["eqA"]
                    qs, qsk = scratch()
                    act(qs, bank[:, :], AF.Silu, [bk], [qsk])
                    stt("dve", QT[:, c, :], qs, 128.0 ** -0.5, eq, ALU.mult, ALU.mult, [qsk, eqk], [("QT", c)])
                elif kind == "c_q":
                    c = idx
                    if True:
                        gbank, gbk = mmbank()
                        mmg(gbank[:, :], [(wgkb[:, l * 256 + c * 128:l * 256 + (c + 1) * 128], crT[:, :])],
                            [("wgkb",), ("crT",)], [gbk])
                        ee, eek = scratch()
                        act(ee, gbank[:, :], AF.Exp, [gbk, ("negbgk",)], [eek], scale=-1.0,
                            bias=negbgk[:, l * 2 + c:l * 2 + c + 1])
                        sp, spk = scratch()
                        ts("dve", ee, ee, 1.0, None, ALU.add, None, [eek], [eek])
                        act(sp, ee, AF.Ln, [eek], [spk])
                        Bt, Bk = scratch()
                        P.op("dve", lambda e, Bt=Bt, sp=sp: e.tensor_tensor_scan(
                            out=Bt, data0=cc("cmask", 0, 512), data1=sp, initial=0.0, op0=ALU.mult, op1=ALU.add),
                            [spk, CST], [Bk])
                        eq, eqk, ek, ekk = gate_generic(Bt, Bk, 6 + c, -1.0 / 16.0)
                        state["eqC"] = (eq, eqk, ek, ekk)
                    eq, eqk, ek, ekk = state["eqC"]
                    qf, qfk = scratch()
                    cp("act", qf, bank[:, :], [bk], [qfk])
                    for hh_ in range(2):
                        stt("dve", QT[:, 8 + 2 * c + hh_, :], qf, cc("hm8", hh_, 1), eq, ALU.mult, ALU.mult,
                            [qfk, eqk, CST], [("QT", 8 + 2 * c + hh_)])
                elif kind == "c_k":
                    c = idx
                    eq, eqk, ek, ekk = state["eqC"]
                    kf, kfk = scratch()
                    cp("act", kf, bank[:, :], [bk], [kfk])
                    tt("pool", KT[:, 6 + c, :], kf, ek, ALU.mult, [kfk, ekk], [("KT", 6 + c)])
                    k2_flush(2)
                    j = nxt("k2t", 3)
                    tt("pool", K2T[:, j, :].rearrange("p (c t) -> p c t", t=64),
                       KT[:, 6 + c, :].rearrange("p (c t) -> p c t", t=64),
                       eq.rearrange("p (c t) -> p c t", t=64)[:, :, 63:64].to_broadcast([128, 8, 64]), ALU.mult,
                       [("KT", 6 + c), eqk], [("K2T", j)])
                    k2_transposes(K2T[:, j, :], ("K2T", j), 6 + c, chm_cols)
                elif kind in ("b_q", "b_k"):
                    t1, t1k = scratch()
                    tt("dve", t1, bank[:, :], cosb[:], ALU.mult, [bk, ("cos",)], [t1k])
                    state["t1"] = (t1, t1k)
                elif kind in ("b_qs", "b_ks"):
                    c = idx
                    t1, t1k = state["t1"]
                    t2, t2k = scratch()
                    tt("dve", t2, bank[:, :], sinb[:], ALU.mult, [bk, ("sin",)], [t2k])
                    tt("pool", t2, t2, t1, ALU.add, [t2k, t1k], [t2k])
                    t23 = t2.rearrange("p (c t) -> p c t", t=64)
                    if kind == "b_qs":
                        for hh_ in range(2):
                            h = 2 * c + hh_
                            tab = cst[:, COFF["eqBz"] + h * 64:COFF["eqBz"] + (h + 1) * 64]
                            tt("pool" if hh_ else "dve", QT[:, 4 + h, :].rearrange("p (c t) -> p c t", t=64), t23,
                               tab.unsqueeze(1).to_broadcast([128, 8, 64]), ALU.mult, [t2k, CST], [("QT", 4 + h)])
                    else:
                        tab = cst[:, COFF["ekB"] + c * 64:COFF["ekB"] + (c + 1) * 64]
                        tt("dve", KT[:, 4 + c, :].rearrange("p (c t) -> p c t", t=64), t23,
                           tab.unsqueeze(1).to_broadcast([128, 8, 64]), ALU.mult, [t2k, CST], [("KT", 4 + c)])
                        k2_flush(2)
                        j = nxt("k2t", 3)
                        ts("dve", K2T[:, j, :], KT[:, 4 + c, :], c2b[:, c:c + 1], None, ALU.mult, None,
                           [("KT", 4 + c), ("c2b",)], [("K2T", j)])
                        k2_transposes(K2T[:, j, :], ("K2T", j), 4 + c, chm_cols)
                elif kind == "mg":
                    act(big[:, idx * 512:(idx + 1) * 512], bank[:, :], AF.Sigmoid, [bk], [("big", idx)])

            k2_flush(0)
            for t_ in range(6):
                ri = use_slot(l, S_TOK + t_)
                for s in range(NSUB):
                    bank, bk = mmbank()
                    mmg(bank[:, :], [(xn[:, kc, s * 128:(s + 1) * 128], ring[:, ri, kc * 512:(kc + 1) * 512])
                                     for kc in range(8)], XN_ALL + [("wr", ri)], [bk])
                    if t_ % 2 == 0:
                        cp("act", tokv[:, s, t_ * 512:(t_ + 1) * 512], bank[:, :], [bk], [("tokv", s, t_)])
                    else:
                        act(tokv[:, s, t_ * 512:(t_ + 1) * 512], bank[:, :], AF.Silu, [bk], [("tokv", s, t_)])

            MIX = [
                ("A", 0, 4), ("B", 4, 2), ("C", 6, 2)]
            y_pend = []
            n_pend = []
            for s in range(NSUB):
                par = s % 2
                for mi, (mname, b0, nb) in enumerate(MIX):
                    vseg = 2 * mi
                    SK = ("S", l, mi)

                    def qidx(h):
                        return h if mi == 0 else (4 + h if mi == 1 else 8 + h)

                    def kblk(h):
                        return h if mi == 0 else b0 + h // 2

                    items = []
                    rd = [("mask4",)]
                    for h in range(4):
                        items.append(("mm", p_sc[:, h * 128:(h + 1) * 128], KT[:, kblk(h), s * 128:(s + 1) * 128],
                                      QT[:, qidx(h), s * 128:(s + 1) * 128], True, True))
                        rd += [("KT", kblk(h)), ("QT", qidx(h))]
                    pe_multi(items, rd, [("sc",)])
                    si = nxt("scm", 2)
                    tt("dve", scm[:, si, :], p_sc[:, :], mask4[:].rearrange("p h i -> p (h i)"), ALU.mult,
                       [("sc",), ("mask4",)], [("scm", si)])
                    for ch in range(2):
                        ci_ = s * 2 + ch
                        erk = [("eref", b0 + j) for j in range(nb)]
                        elk = [("elast", b0 + j) for j in range(nb)]
                        tt("pool", sref[:, ch, b0:b0 + nb, :], S[:, l, b0:b0 + nb, :],
                           eref[:, b0:b0 + nb, ci_:ci_ + 1].to_broadcast([128, nb, 128]), ALU.mult,
                           [SK] + erk, [("sref", ch, mi)])
                        items = []
                        rd = [("tokv", s, vseg)]
                        for h in range(4):
                            if mi == 0:
                                o_ = p_kv[:, h * 128:(h + 1) * 128]
                                l_ = K2tok[:, ch, s, h * 128:(h + 1) * 128]
                            else:
                                p0 = 64 * (h % 2)
                                blk = b0 + h // 2
                                o_ = p_kv[p0:p0 + 64, (h // 2) * 128:(h // 2 + 1) * 128]
                                l_ = K2tok[:, ch, s, blk * 128 + p0:blk * 128 + p0 + 64]
                            items.append(("mm", o_, l_, tokv[:, s, vseg * 512 + h * 128:vseg * 512 + (h + 1) * 128],
                                          True, True))
                            rd.append(("K2tok", ch, kblk(h)))
                        pe_multi(items, rd, [("kv",)])
                        tt("pool", S[:, l, b0:b0 + nb, :], S[:, l, b0:b0 + nb, :],
                           elast[:, b0:b0 + nb, ci_:ci_ + 1].to_broadcast([128, nb, 128]), ALU.mult,
                           [SK] + elk, [SK])
                        tt("dve", S[:, l, b0:b0 + nb, :], S[:, l, b0:b0 + nb, :],
                           p_kv[:, 0:nb * 128].rearrange("p (b v) -> p b v", b=nb), ALU.add, [SK, ("kv",)], [SK])
                    y_flush_after_o = list(y_pend)
                    del y_pend[:]
                    oi = nxt("o", 2)
                    po = p_o[oi]
                    items = []
                    rd = [("scm", si), ("tokv", s, vseg), ("sref", 0, mi), ("sref", 1, mi)]
                    for h in range(4):
                        oc = slice(h * 128, (h + 1) * 128)
                        items.append(("mm", po[:, oc], scm[:, si, oc],
                                      tokv[:, s, vseg * 512 + h * 128:vseg * 512 + (h + 1) * 128], True, False))
                        for ch in range(2):
                            items.append(("mm", po[ch * 64:(ch + 1) * 64, oc],
                                          QT[:, qidx(h), s * 128 + ch * 64:s * 128 + (ch + 1) * 64],
                                          sref[:, ch, kblk(h), :], False, ch == 1))
                        rd.append(("QT", qidx(h)))
                    pe_multi(items, rd, [("o", oi)])
                    for f_ in y_flush_after_o:
                        f_()
                    def norm_chain(s=s, par=par, mi=mi, vseg=vseg, oi=oi, po=po):
                        st, stk = stat()
                        yv = ybuf[:, par, mi * 512:(mi + 1) * 512]
                        yk = ("y", par, mi)
                        gseg = tokv[:, s, (vseg + 1) * 512:(vseg + 2) * 512]
                        P.op("pool", lambda e, st=st: e.memset(st[:, 0:8], 0.0), [], [stk])
                        sq, sqk = scratch()

                        def acc(func, h, scale, col, st=st, sq=sq, po=po):
                            P.op("act", lambda e: e.activation(
                                out=sq[:, h * 128:(h + 1) * 128], in_=po[:, h * 128:(h + 1) * 128], func=func,
                                scale=scale, accum_out=st[:, col:col + 1]), [("o", oi), stk], [sqk, stk])
                        if mi == 1:
                            for h in range(4):
                                acc(AF.Copy, h, 1.0 / 128.0, 4 + h)
                        for h in range(4):
                            acc(AF.Square, h, 128.0 ** -0.5, h)
                        yt, ytk = scratch()
                        if mi != 1:
                            act(st[:, 0:4], st[:, 0:4], AF.Ln, [stk, CST], [stk], bias=cc("eps", 0, 1))
                            act(st[:, 0:4], st[:, 0:4], AF.Exp, [stk], [stk], scale=-0.5)
                            for h in range(4):
                                act(yt[:, h * 128:(h + 1) * 128], po[:, h * 128:(h + 1) * 128], AF.Copy,
                                    [("o", oi), stk], [ytk], scale=st[:, h:h + 1])
                        else:
                            st2, st2k = stat()
                            tt("pool", st2[:, 0:4], st[:, 4:8], st[:, 4:8], ALU.mult, [stk], [st2k])
                            tt("pool", st[:, 0:4], st[:, 0:4], st2[:, 0:4], ALU.subtract, [stk, st2k], [stk])
                            act(st[:, 0:4], st[:, 0:4], AF.Ln, [stk, CST], [stk], bias=cc("eps", 0, 1))
                            act(st[:, 0:4], st[:, 0:4], AF.Exp, [stk], [stk], scale=-0.5)
                            tt("pool", st2[:, 4:8], st[:, 4:8], st[:, 0:4], ALU.mult, [stk], [st2k])
                            act(st2[:, 4:8], st2[:, 4:8], AF.Copy, [st2k], [st2k], scale=-1.0)
                            for h in range(4):
                                act(yt[:, h * 128:(h + 1) * 128], po[:, h * 128:(h + 1) * 128], AF.Identity,
                                    [("o", oi), stk, st2k], [ytk], scale=st[:, h:h + 1], bias=st2[:, 4 + h:5 + h])
                        tt("pool", yv, yt, gseg, ALU.mult, [ytk, ("tokv", s, vseg + 1)], [yk])
                        def ytr(par=par, mi=mi, s=s, yk=yk):
                            half = nxt("tr", 2)
                            items = []
                            for h in range(4):
                                items.append(("tr", p_tr[:, half, h * 128:(h + 1) * 128],
                                              ybuf[:, par, mi * 512 + h * 128:mi * 512 + (h + 1) * 128], identb[:],
                                              True, True))
                            pe_multi(items, [yk, ("identb",)], [("tr",)])
                            src = p_tr[:, half, :].rearrange("p (h t) -> p h t", h=4)
                            dst = yT[:, mi * 4:(mi + 1) * 4, s * 128:(s + 1) * 128]
                            if mi == 0:
                                act(dst, src, AF.Copy, [("tr",), CST], [("yT", mi, s)], scale=cc("gna", l, 1))
                            elif mi == 2:
                                act(dst, src, AF.Copy, [("tr",), CST], [("yT", mi, s)], scale=cc("gnc", l, 1))
                            else:
                                cp("act", dst, src, [("tr",)], [("yT", mi, s)])
                        y_pend.append(ytr)
                    while n_pend:
                        n_pend.pop(0)()
                    n_pend.append(norm_chain)
            while y_pend:
                y_pend.pop(0)()
            while n_pend:
                n_pend.pop(0)()
            while y_pend:
                y_pend.pop(0)()

            rbr = [use_slot(l, S_BR + br) for br in range(3)]
            for dc in range(8):
                accs = []
                for br in range(3):
                    bank, bk = mmbank()
                    mmg(bank[:, :], [(ring[:, rbr[br], (dc * 4 + kc) * 128:(dc * 4 + kc + 1) * 128],
                                      yT[:, br * 4 + kc, :]) for kc in range(4)],
                        [("wr", rbr[br])] + [("yT", br, s) for s in range(NSUB)], [bk])
                    a, ak = scratch()
                    g = br * 8 + dc
                    tt("dve", a, bank[:, :], big[:, g * 512:(g + 1) * 512], ALU.mult, [bk, ("big", g)], [ak])
                    accs.append((a, ak))
                tt("pool", accs[0][0], accs[0][0], accs[1][0], ALU.add, [accs[0][1], accs[1][1]], [accs[0][1]])
                tt("pool", xn[:, dc, :], accs[0][0], accs[2][0], ALU.add, [accs[0][1], accs[2][1]], [("xn", dc)])
            for i in range(2):
                ri = use_slot(l, S_OUT + i)
                for dl in range(4):
                    dc = 4 * i + dl
                    bank, bk = mmbank()
                    mmg(bank[:, :], [(ring[:, ri, (dl * 8 + kc) * 128:(dl * 8 + kc + 1) * 128], xn[:, kc, :])
                                     for kc in range(8)], XN_ALL + [("wr", ri)], [bk])
                    tt("dve", xT[:, dc, :], xT[:, dc, :], bank[:, :], ALU.add, [("xT", dc), bk], [("xT", dc)])

            rmsnorm_to_xn("nffn", l)
            for i in range(11):
                ri = use_slot(l, S_FFN + i)
                for j in range(2):
                    fc = 2 * i + j
                    bg, bgk_ = mmbank()
                    mmg(bg[:, :], [(ring[:, ri, (2 * j * 8 + kc) * 128:(2 * j * 8 + kc + 1) * 128], xn[:, kc, :])
                                   for kc in range(8)], XN_ALL + [("wr", ri)], [bgk_])
                    bu, buk = mmbank()
                    mmg(bu[:, :], [(ring[:, ri, ((2 * j + 1) * 8 + kc) * 128:((2 * j + 1) * 8 + kc + 1) * 128],
                                    xn[:, kc, :]) for kc in range(8)], XN_ALL + [("wr", ri)], [buk])
                    sg, sgk = scratch()
                    act(sg, bg[:, :], AF.Silu, [bgk_], [sgk])
                    tt("dve", big[:, fc * 512:(fc + 1) * 512], sg, bu[:, :], ALU.mult, [sgk, buk], [("big", fc)])
            for dc in range(8):
                ri = use_slot(l, S_DOWN + dc)
                bank, bk = mmbank()
                mmg(bank[:, :], [(ring[:, ri, kc * 128:(kc + 1) * 128], big[:, kc * 512:(kc + 1) * 512])
                                 for kc in range(FC)], [("big", kc) for kc in range(FC)] + [("wr", ri)], [bk])
                tt("dve", xT[:, dc, :], xT[:, dc, :], bank[:, :], ALU.add, [("xT", dc), bk], [("xT", dc)])

        identf = cc("ident", 0, 128)
        xio = big[:, 0:2 * SLOTW].bitcast(F32).rearrange("p (s d) -> p s d", s=NSUB)
        xio_keys = [("big", u) for u in range(16)]
        xout = tokv[:].rearrange("p s c -> p (s c)")[:, 0:2 * SLOTW].bitcast(F32).rearrange("p (s d) -> p s d", s=NSUB)

        def xout_keys(s, hf):
            e0 = s * 2048 + hf * 1024
            return sorted({("tokv", e // 3072, (e % 3072) // 512) for e in range(e0, e0 + 1024, 512)})
        xout_all = sorted({k for s in range(NSUB) for hf in range(2) for k in xout_keys(s, hf)})
        for sq_ in range(n_seq):
            for l in range(depth):
                P.op("pool", lambda e, l=l: e.memset(S[:, l, :, :], 0.0), [], [("S", l, 0), ("S", l, 1), ("S", l, 2)])
            for tl in range(n_tiles):
                t0 = sq_ * seq_t + tl * T
                dma("sp", xio, x_d[t0:t0 + T, :].rearrange("(s p) d -> p s d", p=128), "xio", [], xio_keys)
                dma("sp", cosb[:], cos_d[:, tl * T:(tl + 1) * T], "cos", [], [("cos",)])
                dma("sp", sinb[:], sin_d[:, tl * T:(tl + 1) * T], "sin", [], [("sin",)])
                for dc in range(8):
                    bank, bk = mmbank()
                    items = [("tr", bank[:, s * 128:(s + 1) * 128], xio[:, s, dc * 128:(dc + 1) * 128], identf,
                              True, True) for s in range(NSUB)]
                    pe_multi(items, xio_keys + [CST], [bk])
                    cp("act" if dc % 2 else "dve", xT[:, dc, :], bank[:, :], [bk], [("xT", dc)])
                for l in range(depth):
                    layer(l, tl)
                for dc in range(8):
                    if dc < 5:
                        act(xn[:, dc, :], xT[:, dc, :], AF.Square, [("xT", dc)], [("xn", dc)])
                    else:
                        tt("pool", xn[:, dc, :], xT[:, dc, :], xT[:, dc, :], ALU.mult, [("xT", dc)], [("xn", dc)])
                bank, bk = mmbank()
                mmg(bank[:, :], [(onesb[:], xn[:, dc, :]) for dc in range(8)], XN_ALL + [("onesb",)], [bk])
                act(rstd[:], bank[:, :], AF.Ln, [bk, CST], [("rstd",)], scale=1.0 / D, bias=cc("eps", 0, 1))
                act(rstd[:], rstd[:], AF.Exp, [("rstd",)], [("rstd",)], scale=-0.5)
                for dc in range(8):
                    stt("dve", xT[:, dc, :], xT[:, dc, :], cc("nfin", dc, 1), rstd[:],
                        ALU.mult, ALU.mult, [("xT", dc), ("rstd",), CST], [("xT", dc)])
                for s in range(NSUB):
                    for hf in range(2):
                        bank, bk = mmbank()
                        items = [("tr", bank[:, j * 128:(j + 1) * 128], xT[:, hf * 4 + j, s * 128:(s + 1) * 128],
                                  identf, True, True) for j in range(4)]
                        pe_multi(items, [("xT", hf * 4 + j) for j in range(4)] + [CST], [bk])
                        cp("act" if hf else "dve", xout[:, s, hf * 512:(hf + 1) * 512], bank[:, :], [bk],
                           xout_keys(s, hf))
                dma("act", out_d[t0:t0 + T, :].rearrange("(s p) d -> p s d", p=128), xout, "xout", xout_all, [])

        P.finalize(getsem)
        block = es.enter_context(nc.Block())

        @block.tensor
        def _(e):
            P.emit_engine("pe", e)

        @block.scalar
        def _(e):
            P.emit_engine("act", e)

        @block.vector
        def _(e):
            P.emit_engine("dve", e)

        @block.gpsimd
        def _(e):
            P.emit_engine("pool", e)

        @block.sync
        def _(e):
            P.emit_engine("sp", e, final_waits=True)
    return nc


def run(inputs, n_seq, n_tiles, depth):
    import time
    t0 = time.time()
    x = np.ascontiguousarray(inputs["x"], dtype=np.float32)
    seq_t = n_tiles * T
    cst, extra = build_consts(inputs)
    wsrc = build_wsrc(inputs, depth)
    cosT, sinT = rope_tables(seq_t)
    print("[kernel] host prep %.1fs" % (time.time() - t0), flush=True)
    t0 = time.time()
    nc = build_program(n_seq, n_tiles, depth)
    print("[kernel] build %.1fs" % (time.time() - t0), flush=True)
    t0 = time.time()
    in_maps = []
    for c in range(NCORES):
        xc = x[c * n_seq:(c + 1) * n_seq].reshape(n_seq * seq_t, D)
        in_maps.append({"x": xc, "wsrc": wsrc, "cst": cst, "c2b": extra["c2B"], "wgk": extra["wgk"], "cosT": cosT, "sinT": sinT})
    ncr = _TEST_CORES or NCORES
    res = run_bass_kernel_spmd(nc, in_maps[:ncr], core_ids=list(range(ncr)))
    print("[kernel] compile+run %.1fs" % (time.time() - t0), flush=True)
    outs = [np.asarray(r["out"]).reshape(n_seq, seq_t, D) for r in res.results]
    while len(outs) < NCORES:
        outs.append(np.zeros_like(outs[0]))
    return np.concatenate(outs, axis=0).astype(np.float32)


def kernel(**inputs):
    return run(inputs, BATCH // NCORES, SEQ // T, DEPTH)
```

```python
import numpy as np
from contextlib import ExitStack
import concourse.bass as bass
import concourse.mybir as mybir
from concourse.bass_utils import run_bass_kernel_spmd

F32 = mybir.dt.float32
BF = mybir.dt.bfloat16
AF = mybir.ActivationFunctionType
ALU = mybir.AluOpType
AX = mybir.AxisListType

D = 1024
DEPTH = 4
SEQ = 4096
BATCH = 16
NCORES = 8
_TEST_CORES = 0
T = 512
NSUB = 4
KC = 8
FFN = 2816
FC = FFN // 128
PROJ = 8208
EPS = 1e-6
NBUF = 3
NSCR = 5
SLOTW = 4096
CLAMP = 40.0

C_AQ, C_AF, C_AI, C_AG = 0, 512, 1024, 1536
C_BQ, C_BK, C_BV, C_BG = 2048, 2304, 2560, 3072
C_CQ, C_CK, C_CV, C_CG, C_CR = 3584, 3840, 4096, 4608, 5120
C_MG = 5136


def _swap_cols(c0, n):
    idx = np.arange(n)
    return c0 + (idx ^ 1)


def fm_chunk_list():
    L = [("c_r", 0, np.arange(C_CR, C_CR + 16))]
    for c in range(4):
        L.append(("a_f", c, np.arange(C_AF + 128 * c, C_AF + 128 * (c + 1))))
        L.append(("a_q", c, np.arange(C_AQ + 128 * c, C_AQ + 128 * (c + 1))))
    for c in range(2):
        L.append(("c_q", c, np.arange(C_CQ + 128 * c, C_CQ + 128 * (c + 1))))
        L.append(("c_k", c, np.arange(C_CK + 128 * c, C_CK + 128 * (c + 1))))
    for c in range(2):
        L.append(("b_q", c, np.arange(C_BQ + 128 * c, C_BQ + 128 * (c + 1))))
        L.append(("b_qs", c, _swap_cols(C_BQ + 128 * c, 128)))
        L.append(("b_k", c, np.arange(C_BK + 128 * c, C_BK + 128 * (c + 1))))
        L.append(("b_ks", c, _swap_cols(C_BK + 128 * c, 128)))
    for g in range(24):
        L.append(("mg", g, np.arange(C_MG + 128 * g, C_MG + 128 * (g + 1))))
    return L


FM = fm_chunk_list()
NFM_SLOTS = (len(FM) + 3) // 4
TOK_COLS = [C_AI, C_AG, C_BV, C_BG, C_CV, C_CG]
S_TOK = NFM_SLOTS
S_BR = S_TOK + 6
S_OUT = S_BR + 3
S_FFN = S_OUT + 2
S_DOWN = S_FFN + 11
NSLOT = S_DOWN + 8


def slot_width(slot):
    if slot < NFM_SLOTS:
        n = min(4, len(FM) - 4 * slot)
        return n * 1024
    if slot >= S_DOWN:
        return FC * 128
    return SLOTW


def const_layout():
    off = {}
    cur = [0]

    def add(name, n):
        off[name] = cur[0]
        cur[0] += n

    add("nmix", DEPTH * 8)
    add("nffn", DEPTH * 8)
    add("nfin", 8)
    add("lbl", DEPTH * 4)
    add("bgk", DEPTH * 2)
    add("gna", DEPTH)
    add("gnc", DEPTH)
    add("ident", 128)
    add("mask", 128)
    add("cmask", 512)
    add("eqBz", 4 * 64)
    add("ekB", 2 * 64)
    add("erefB", 2)
    add("elastB", 2)
    add("chm", 2)
    add("chmc2B", 4)
    add("hm8", 2)
    add("eps", 1)
    return off, cur[0]


COFF, NCONST = const_layout()


def build_consts(inp):
    c = np.zeros((128, NCONST), np.float32)
    p = np.arange(128)

    def put(name, arr):
        arr = np.asarray(arr, np.float32)
        c[:, COFF[name]:COFF[name] + arr.shape[1]] = arr

    def fm(v):
        L = v.shape[0]
        n = v.shape[1] // 128
        return np.ascontiguousarray(v.reshape(L, n, 128).transpose(2, 0, 1).reshape(128, L * n))

    put("nmix", fm(inp["norm_mix"]))
    put("nffn", fm(inp["norm_ffn"]))
    put("nfin", fm(inp["norm_final"][None, :]))
    put("lbl", fm(inp["lb_logits"]))
    put("bgk", fm(inp["b_gk"]))
    put("gna", fm(inp["gn_a"]))
    put("gnc", fm(inp["gn_c"]))
    put("ident", np.eye(128, dtype=np.float32))
    j = p[:, None]
    i = p[None, :]
    put("mask", ((j <= i) & ((j // 64) == (i // 64))).astype(np.float32))
    t = np.arange(512)
    put("cmask", np.broadcast_to((t % 64 != 0).astype(np.float32)[None, :], (128, 512)))
    lg = np.log(1.0 - 2.0 ** (-5.0 - np.arange(4, dtype=np.float64)))
    tt = np.arange(64, dtype=np.float64)
    eqBz = np.zeros((128, 4 * 64))
    ekB = np.zeros((128, 2 * 64))
    erefB = np.zeros((128, 2))
    elastB = np.zeros((128, 2))
    c2B = np.zeros((128, 2))
    for h in range(4):
        cch, half = h // 2, h % 2
        rows = slice(64 * half, 64 * half + 64)
        eqBz[rows, h * 64:(h + 1) * 64] = np.exp(lg[h] * (tt - 32.0))[None, :]
        ekB[rows, cch * 64:(cch + 1) * 64] = (np.exp(-lg[h] * (tt - 32.0)) * 0.125)[None, :]
        erefB[rows, cch] = np.exp(lg[h] * 33.0)
        elastB[rows, cch] = np.exp(lg[h] * 64.0)
        c2B[rows, cch] = np.exp(lg[h] * 31.0)
    put("eqBz", eqBz)
    put("ekB", ekB)
    put("erefB", erefB)
    put("elastB", elastB)
    chm = np.zeros((128, 2))
    chm[:64, 0] = 1.0
    chm[64:, 1] = 1.0
    put("chm", chm)
    chmc2B = np.zeros((128, 4))
    for cch in range(2):
        for ch in range(2):
            chmc2B[:, cch * 2 + ch] = chm[:, ch] * 1.0
    put("chmc2B", chmc2B)
    hm8 = np.zeros((128, 2))
    hm8[:64, 0] = 0.125
    hm8[64:, 1] = 0.125
    put("hm8", hm8)
    wgk = np.ascontiguousarray(inp["w_gk_up"].transpose(1, 0, 2).reshape(16, DEPTH * 256), dtype=np.float32)
    put("eps", np.full((128, 1), EPS, np.float32))
    extra = {"c2B": c2B.astype(np.float32), "wgk": wgk}
    return c, extra


def rope_tables(seq):
    inv_freq = (1.0 / (10000.0 ** np.linspace(0.0, 1.0, 32, dtype=np.float32))).astype(np.float32)
    ang = np.arange(seq, dtype=np.float32)[:, None] * inv_freq[None, :]
    sin, cos = np.sin(ang), np.cos(ang)
    f = np.arange(128) % 64
    cosT = cos[:, f // 2].T.astype(np.float32)
    sgn = np.where(f % 2 == 0, -1.0, 1.0).astype(np.float32)
    sinT = (sin[:, f // 2].T * sgn[:, None]).astype(np.float32)
    return np.ascontiguousarray(cosT), np.ascontiguousarray(sinT)


def build_wsrc(inp, depth):
    w = np.zeros((depth, NSLOT, 128, SLOTW), np.float32)

    def kmaj(m):
        K = m.shape[0] // 128
        return m.reshape(K, 128, m.shape[1]).transpose(1, 0, 2)

    for l in range(depth):
        win = inp["w_in"][l]
        for ci, (kind, idx, cols) in enumerate(FM):
            slot, pos = ci // 4, ci % 4
            blk = kmaj(win[:, cols])
            w[l, slot].reshape(128, 4, 8, 128)[:, pos, :, :blk.shape[2]] = blk
        for t_, c0 in enumerate(TOK_COLS):
            w[l, S_TOK + t_] = kmaj(win[:, c0:c0 + 512]).reshape(128, SLOTW)
        for br, nm in enumerate(("w_br_a", "w_br_b", "w_br_c")):
            wb = kmaj(inp[nm][l])
            dst = w[l, S_BR + br].reshape(128, 8, 4, 128)
            dst[:] = wb.reshape(128, 4, 8, 128).transpose(0, 2, 1, 3)
        wo = kmaj(inp["w_out"][l])
        for i in range(2):
            dst = w[l, S_OUT + i].reshape(128, 4, 8, 128)
            dst[:] = wo[:, :, i * 512:(i + 1) * 512].reshape(128, 8, 4, 128).transpose(0, 2, 1, 3)
        wg = kmaj(inp["w_ffn_gate"][l])
        wu = kmaj(inp["w_ffn_up"][l])
        for i in range(11):
            dst = w[l, S_FFN + i].reshape(128, 4, 8, 128)
            for j in range(2):
                fc = 2 * i + j
                dst[:, 2 * j] = wg[:, :, fc * 128:(fc + 1) * 128]
                dst[:, 2 * j + 1] = wu[:, :, fc * 128:(fc + 1) * 128]
        wd = kmaj(inp["w_ffn_down"][l])
        for dc in range(8):
            w[l, S_DOWN + dc, :, :FC * 128] = wd[:, :, dc * 128:(dc + 1) * 128].reshape(128, FC * 128)
    return w


ENGS = ("pe", "act", "dve", "pool", "sp")


class Op:
    __slots__ = ("eng", "fn", "deps", "needed", "cnt", "sem", "dma")

    def __init__(self, eng, fn, dma):
        self.eng = eng
        self.fn = fn
        self.deps = ()
        self.needed = False
        self.cnt = 0
        self.sem = None
        self.dma = dma


class Prog:
    def __init__(self):
        self.q = {e: [] for e in ENGS}
        self.last_w = {}
        self.readers = {}
        self.dma_keys = {}

    def op(self, eng, fn, reads=(), writes=(), dma_key=None):
        o = Op(eng, fn, dma_key)
        deps = set()
        lw, rd = self.last_w, self.readers
        for r in reads:
            w = lw.get(r)
            if w is not None:
                deps.add(w)
        for r in writes:
            w = lw.get(r)
            if w is not None:
                deps.add(w)
            rs = rd.get(r)
            if rs:
                deps.update(rs)
        for r in reads:
            rd.setdefault(r, []).append(o)
        for r in writes:
            lw[r] = o
            rd[r] = []
        o.deps = tuple(deps)
        for d in deps:
            d.needed = True
        self.q[eng].append(o)
        if dma_key is not None:
            self.dma_keys[dma_key] = None
        return o

    def finalize(self, getsem):
        self.eng_sem = {e: getsem("c_" + e) for e in ENGS}
        self.dma_sem = {k: getsem("d_%d" % i) for i, k in enumerate(self.dma_keys)}
        self.dma_cnt = {k: 0 for k in self.dma_keys}
        for e in ENGS:
            c = 0
            for o in self.q[e]:
                if o.dma is not None:
                    self.dma_cnt[o.dma] += 16
                    o.sem = self.dma_sem[o.dma]
                    o.cnt = self.dma_cnt[o.dma]
                elif o.needed:
                    c += 1
                    o.sem = self.eng_sem[e]
                    o.cnt = c

    def emit_engine(self, e, engobj, final_waits=False):
        waited = {}
        for o in self.q[e]:
            need = {}
            for d in o.deps:
                if e == "pe" and d.eng == "pe" and d.dma is None:
                    continue
                s = d.sem
                k = id(s)
                if need.get(k, (None, 0))[1] < d.cnt:
                    need[k] = (s, d.cnt)
            for k, (s, c) in need.items():
                if waited.get(k, 0) >= c:
                    continue
                engobj.wait_ge(s, c)
                waited[k] = c
            ins = o.fn(engobj)
            if o.dma is not None:
                ins.then_inc(o.sem, 16)
            elif o.needed:
                ins.then_inc(o.sem, 1)
        if final_waits:
            for k, s in self.dma_sem.items():
                if self.dma_cnt[k] > 0:
                    engobj.wait_ge(s, self.dma_cnt[k])


def build_program(n_seq, n_tiles, depth, debug=False):
    seq_t = n_tiles * T
    ntok = n_seq * seq_t
    nc = bass.Bass("TRN2", target_bir_lowering=False)
    x_d = nc.dram_tensor("x", [ntok, D], F32, kind="ExternalInput").ap()
    wsrc_d = nc.dram_tensor("wsrc", [depth, NSLOT, 128, SLOTW], F32, kind="ExternalInput").ap()
    cst_d = nc.dram_tensor("cst", [128, NCONST], F32, kind="ExternalInput").ap()
    c2b_d = nc.dram_tensor("c2b", [128, 2], F32, kind="ExternalInput").ap()
    wgk_d = nc.dram_tensor("wgk", [16, DEPTH * 256], F32, kind="ExternalInput").ap()
    cos_d = nc.dram_tensor("cosT", [128, seq_t], F32, kind="ExternalInput").ap()
    sin_d = nc.dram_tensor("sinT", [128, seq_t], F32, kind="ExternalInput").ap()
    out_d = nc.dram_tensor("out", [ntok, D], F32, kind="ExternalOutput").ap()
    wst_d = nc.dram_tensor("wst", [depth, NSLOT, 128, SLOTW], BF, kind="Internal").ap()

    P = Prog()
    es = ExitStack()
    with es:
        def sb(name, shape, dt):
            return es.enter_context(nc.sbuf_tensor("s_" + name, shape, dt))

        def ps(name, shape, dt):
            return es.enter_context(nc.psum_tensor(name, shape, dt))

        xT = sb("xT", [128, 8, T], F32)
        xn = sb("xn", [128, 8, T], BF)
        rstd = sb("rstd", [128, T], F32)
        big = sb("big", [128, 24 * 512], BF)
        tokv = sb("tokv", [128, NSUB, 3072], BF)
        ring = sb("ring", [128, NBUF, SLOTW], BF)
        QT = sb("QT", [128, 12, T], BF)
        KT = sb("KT", [128, 8, T], BF)
        K2T = sb("K2T", [128, 3, T], BF)
        K2tok = sb("K2tok", [128, 2, NSUB, 1024], BF)
        scm = sb("scm", [128, 2, 512], BF)
        ybuf = sb("ybuf", [128, 2, 1536], BF)
        yT = sb("yT", [128, 12, T], BF)
        S = sb("S", [128, depth, 8, 128], F32)
        sref = sb("sref", [128, 2, 8, 128], BF)
        eref = sb("eref", [128, 8, 8], F32)
        elast = sb("elast", [128, 8, 8], F32)
        scr = sb("scr", [128, NSCR, T], F32)
        stt_ = sb("stt", [128, 8, 8], F32)
        cst = sb("cst", [128, NCONST], F32)
        c2b = sb("c2b", [128, 2], F32)
        cosb = sb("cosb", [128, T], F32)
        sinb = sb("sinb", [128, T], F32)
        crT = sb("crT", [16, T], BF)
        identb = sb("identb", [128, 128], BF)
        onesb = sb("onesb", [128, 128], BF)
        mask4 = sb("mask4", [128, 4, 128], BF)
        wgkb = sb("wgkb", [16, depth * 256], BF)
        lbt = sb("lbt", [128, 3, depth * 4], F32)
        lbw = sb("lbw", [128, 4, 8], F32)
        negbgk = sb("negbgk", [128, depth * 2], F32)

        mm = [ps("mm%d" % i, [128, 512], F32) for i in range(3)]
        p_sc = ps("p_sc", [128, 512], F32)
        p_o = [ps("p_o%d" % i, [128, 512], F32) for i in range(2)]
        p_kv = ps("p_kv", [128, 512], F32)
        p_tr = ps("p_tr", [128, 2, 512], BF)

        sems = {}

        def getsem(name):
            if name not in sems:
                sems[name] = es.enter_context(nc.semaphore(name))
            return sems[name]

        def dma(eng, out, in_, key, reads, writes):
            P.op(eng, lambda e: e.dma_start(out=out, in_=in_), reads, writes, dma_key=key)

        def act(out, in_, func, reads, writes, bias=None, scale=None):
            kw = {}
            if bias is not None:
                kw["bias"] = bias
            if scale is not None:
                kw["scale"] = scale
            P.op("act", lambda e: e.activation(out=out, in_=in_, func=func, **kw), reads, writes)

        def tt(eng, out, in0, in1, op, reads, writes):
            P.op(eng, lambda e: e.tensor_tensor(out=out, in0=in0, in1=in1, op=op), reads, writes)

        def ts(eng, out, in0, s1, s2, op0, op1, reads, writes):
            if op1 is None:
                P.op(eng, lambda e: e.tensor_scalar(out=out, in0=in0, scalar1=s1, scalar2=None, op0=op0),
                     reads, writes)
            else:
                P.op(eng, lambda e: e.tensor_scalar(out=out, in0=in0, scalar1=s1, scalar2=s2, op0=op0, op1=op1),
                     reads, writes)

        def stt(eng, out, in0, scalar, in1, op0, op1, reads, writes):
            P.op(eng, lambda e: e.scalar_tensor_tensor(out=out, in0=in0, scalar=scalar, in1=in1, op0=op0, op1=op1),
                 reads, writes)

        def cp(eng, out, in_, reads, writes):
            if eng == "act":
                act(out, in_, AF.Copy, reads, writes)
            else:
                P.op(eng, lambda e: e.tensor_copy(out=out, in_=in_), reads, writes)

        def mmg(out, pairs, reads, writes):
            n = len(pairs)

            def fn(e):
                ins = None
                for i, (l_, r_) in enumerate(pairs):
                    ins = e.matmul(out, l_, r_, start=(i == 0), stop=(i == n - 1))
                return ins
            P.op("pe", fn, reads, writes)

        def pe_multi(items, reads, writes):
            def fn(e):
                ins = None
                for (kind, o_, a_, b_, st, sp_) in items:
                    if kind == "mm":
                        ins = e.matmul(o_, a_, b_, start=st, stop=sp_)
                    else:
                        ins = e.transpose(o_, a_, b_)
                return ins
            P.op("pe", fn, reads, writes)

        rot = {"mm": 0, "scr": 0, "o": 0, "scm": 0, "k2t": 0, "tr": 0, "st": 0, "w": 0, "cast": 0}

        def nxt(name, n):
            v = rot[name]
            rot[name] = (v + 1) % n
            return v

        MM_FULL = [(mm[0], ("mm", 0)), (mm[1], ("mm", 1)), (mm[2], ("mm", 2)), (p_sc, ("sc",)),
                   (p_o[0], ("o", 0)), (p_o[1], ("o", 1)), (p_kv, ("kv",))]

        O_BANKS = [p_o[0], p_o[1], mm[2]]
        O_KEYS = [("o", 0), ("o", 1), ("mm", 2)]

        def mmbank():
            i = nxt("mm", len(MM_FULL))
            return MM_FULL[i]

        def scratch():
            i = nxt("scr", NSCR)
            return scr[:, i, :], ("scr", i)

        def stat():
            i = nxt("st", 8)
            return stt_[:, i, :], ("st", i)

        BIG_ALL = [("big", u) for u in range(24)]
        TOKV_ALL = [("tokv", s, g) for s in range(NSUB) for g in range(6)]
        CST = ("cst",)

        def cc(name, i=0, n=1):
            o = COFF[name] + i
            return cst[:, o:o + n]

        dma("sp", cst[:], cst_d, "cst", [], [CST])
        dma("sp", c2b[:], c2b_d, "c2b", [], [("c2b",)])
        cp("dve", identb[:], cc("ident", 0, 128), [CST], [("identb",)])
        P.op("dve", lambda e: e.memset(onesb[:], 1.0), [], [("onesb",)])
        for h in range(4):
            cp("dve", mask4[:, h, :], cc("mask", 0, 128), [CST], [("mask4",)])
        dma("pool", wgkb[:], wgk_d[:, 0:depth * 256], "wgkb", [], [("wgkb",)])
        ts("dve", negbgk[:], cc("bgk", 0, depth * 2), -1.0, None, ALU.mult, None, [CST], [("negbgk",)])
        cp("dve", eref[:, 4:6, :], cc("erefB", 0, 2).unsqueeze(2).to_broadcast([128, 2, 8]), [CST],
           [("eref", 4), ("eref", 5)])
        cp("dve", elast[:, 4:6, :], cc("elastB", 0, 2).unsqueeze(2).to_broadcast([128, 2, 8]), [CST],
           [("elast", 4), ("elast", 5)])
        lbl3 = cst[:, COFF["lbl"]:COFF["lbl"] + 16].rearrange("p (l f) -> p l f", l=4)
        act(lbw[:, :, 0:4], lbl3, AF.Exp, [CST], [("lbw",)])
        P.op("dve", lambda e: e.tensor_reduce(out=lbw[:, 0, 4:8], in_=lbw[:, :, 0:4].rearrange("p l f -> p f l"),
                                              axis=AX.X, op=ALU.add), [("lbw",)], [("lbw2",)])
        P.op("dve", lambda e: e.reciprocal(out=lbw[:, 1, 4:8], in_=lbw[:, 0, 4:8]), [("lbw2",)], [("lbw3",)])
        tt("dve", lbw[:, :, 0:4], lbw[:, :, 0:4], lbw[:, 1:2, 4:8].to_broadcast([128, 4, 4]), ALU.mult,
           [("lbw",), ("lbw3",)], [("lbw",)])
        LBK = ("lbt",)
        P.op("dve", lambda e: e.memset(lbt[:, 0, 0:4], 0.0), [], [LBK])
        for l in range(1, depth):
            tt("dve", lbt[:, 0, 4 * l:4 * l + 4], lbt[:, 0, 4 * l - 4:4 * l], lbw[:, l, 0:4], ALU.add,
               [LBK, ("lbw",)], [LBK])
        ts("dve", lbt[:, 1, :], lbt[:, 0, :], -1.0, 1.0, ALU.mult, ALU.add, [LBK], [("lbt1",)])
        ts("dve", lbt[:, 2, :], lbt[:, 0, :], 1.0, -1.0, ALU.mult, ALU.add, [LBK], [("lbt2",)])
        LBR = [LBK, ("lbt1",), ("lbt2",)]

        st32 = [tokv[:].rearrange("p s c -> p (s c)")[:, 0:2 * SLOTW].bitcast(F32),
                big[:, 0:2 * SLOTW].bitcast(F32),
                xT[:].rearrange("p a b -> p (a b)")]
        st32_keys = [TOKV_ALL, BIG_ALL, [("xT", dc) for dc in range(8)]]
        cast_engs = ["dve", "act"]
        k = 0
        for l in range(depth):
            for slot in range(NSLOT):
                w_ = slot_width(slot)
                i = k % 3
                dma("sp", st32[i][:, :w_], wsrc_d[l, slot, :, :w_], ("st32", i), [], st32_keys[i])
                ce = cast_engs[k % 2]
                cp(ce, ring[:, i, :w_], st32[i][:, :w_], st32_keys[i], [("wr", i)])
                dma("sp", wst_d[l, slot, :, :w_], ring[:, i, :w_], ("st16", i), [("wr", i)], [("wst", l, slot)])
                k += 1

        wcount = [0]

        def use_slot(l, slot):
            ri = wcount[0] % NBUF
            wcount[0] += 1
            w_ = slot_width(slot)
            dma("sp", ring[:, ri, :w_], wst_d[l, slot, :, :w_], ("wr", ri), [("wst", l, slot)], [("wr", ri)])
            return ri

        XT_ALL = [("xT", dc) for dc in range(8)]
        XN_ALL = [("xn", dc) for dc in range(8)]

        def rmsnorm_to_xn(wname, wl):
            for dc in range(8):
                if dc < 5:
                    act(xn[:, dc, :], xT[:, dc, :], AF.Square, [("xT", dc)], [("xn", dc)])
                else:
                    tt("pool", xn[:, dc, :], xT[:, dc, :], xT[:, dc, :], ALU.mult, [("xT", dc)], [("xn", dc)])
            bank, bk = mmbank()
            mmg(bank[:, :], [(onesb[:], xn[:, dc, :]) for dc in range(8)], XN_ALL + [("onesb",)], [bk])
            act(rstd[:], bank[:, :], AF.Ln, [bk, CST], [("rstd",)], scale=1.0 / D, bias=cc("eps", 0, 1))
            act(rstd[:], rstd[:], AF.Exp, [("rstd",)], [("rstd",)], scale=-0.5)
            for dc in range(8):
                eng = "dve"
                stt(eng, xn[:, dc, :], xT[:, dc, :], cc(wname, wl * 8 + dc, 1), rstd[:], ALU.mult, ALU.mult,
                    [("xT", dc), ("rstd",), CST], [("xn", dc)])

        def gate_generic(Bt, Bk, blk, gs):
            B3 = Bt.rearrange("p (c t) -> p c t", t=64)
            act(eref[:, blk, :], B3[:, :, 32], AF.Exp, [Bk], [("eref", blk)], scale=gs)
            act(elast[:, blk, :], B3[:, :, 63], AF.Exp, [Bk], [("elast", blk)], scale=gs)
            Bc, Bck = scratch()
            Bc3 = Bc.rearrange("p (c t) -> p c t", t=64)
            tt("dve", Bc3, B3, B3[:, :, 32:33].to_broadcast([128, 8, 64]), ALU.subtract, [Bk], [Bck])
            lim = CLAMP / abs(gs)
            ts("dve", Bc, Bc, -lim, lim, ALU.max, ALU.min, [Bck], [Bck])
            eq, eqk = scratch()
            ek, ekk = scratch()
            act(eq, Bc, AF.Exp, [Bck], [eqk], scale=gs)
            act(ek, Bc, AF.Exp, [Bck], [ekk], scale=-gs)
            return eq, eqk, ek, ekk

        k2_pend = []

        def k2_transposes(src, srck, blk, scale_cols):
            k2_pend.append((src, srck, blk, scale_cols))

        def k2_flush(keep):
            while len(k2_pend) > keep:
                k2_transposes_now(*k2_pend.pop(0))

        def k2_transposes_now(src, srck, blk, scale_cols):
            half = nxt("tr", 2)
            items = []
            for s in range(NSUB):
                items.append(("tr", p_tr[:, half, s * 128:(s + 1) * 128], src[:, s * 128:(s + 1) * 128], identb[:],
                              True, True))
            pe_multi(items, [srck, ("identb",)], [("tr",)])
            for ch in range(2):
                act(K2tok[:, ch, :, blk * 128:(blk + 1) * 128],
                    p_tr[:, half, :].rearrange("p (s d) -> p s d", s=NSUB), AF.Copy,
                    [("tr",), CST], [("K2tok", ch, blk)], scale=scale_cols[ch])

        chm_cols = [cc("chm", 0, 1), cc("chm", 1, 1)]

        def layer(l, tile_idx):
            rmsnorm_to_xn("nmix", l)
            ring_of = {}
            state = {}
            for ci, (kind, idx, cols) in enumerate(FM):
                slot, pos = ci // 4, ci % 4
                if pos == 0:
                    ring_of[slot] = use_slot(l, slot)
                ri = ring_of[slot]
                M = len(cols)
                bank, bk = mmbank()

                def grp(bank=bank, bk=bk, ri=ri, pos=pos, M=M):
                    mmg(bank[0:M, :], [(ring[:, ri, (pos * 8 + kc) * 128:(pos * 8 + kc) * 128 + M], xn[:, kc, :])
                                       for kc in range(8)], XN_ALL + [("wr", ri)], [bk])
                grp()
                if kind == "c_r":
                    cp("act", crT[:, :], bank[0:16, :], [bk], [("crT",)])
                elif kind == "a_f":
                    c = idx
                    sig, sigk = scratch()
                    act(sig, bank[:, :], AF.Sigmoid, [bk], [sigk])
                    lf, lfk = scratch()
                    act(lf, sig, AF.Ln, [sigk] + LBR, [lfk], scale=lbt[:, 1, 4 * l + c:4 * l + c + 1],
                        bias=lbt[:, 0, 4 * l + c:4 * l + c + 1])
                    ka, kak = scratch()
                    ts("dve", ka, sig, lbt[:, 2, 4 * l + c:4 * l + c + 1], lbt[:, 1, 4 * l + c:4 * l + c + 1],
                       ALU.mult, ALU.add, [sigk] + LBR, [kak])
                    Bt, Bk = scratch()
                    P.op("dve", lambda e, Bt=Bt, lf=lf: e.tensor_tensor_scan(
                        out=Bt, data0=cc("cmask", 0, 512), data1=lf, initial=0.0, op0=ALU.mult, op1=ALU.add),
                        [lfk, CST], [Bk])
                    eq, eqk, ek, ekk = gate_generic(Bt, Bk, c, 1.0)
                    tt("pool", KT[:, c, :], ka, ek, ALU.mult, [kak, ekk], [("KT", c)])
                    k2_flush(2)
                    j = nxt("k2t", 3)
                    tt("pool", K2T[:, j, :].rearrange("p (c t) -> p c t", t=64),
                       KT[:, c, :].rearrange("p (c t) -> p c t", t=64),
                       eq.rearrange("p (c t) -> p c t", t=64)[:, :, 63:64].to_broadcast([128, 8, 64]), ALU.mult,
                       [("KT", c), eqk], [("K2T", j)])
                    k2_transposes(K2T[:, j, :], ("K2T", j), c, chm_cols)
                    state["eqA"] = (eq, eqk)
                elif kind == "a_q":
                    c = idx
                    eq, eqk = state["eqA"]
                    qs, qsk = scratch()
                    act(qs, bank[:, :], AF.Silu, [bk], [qsk])
                    stt("dve", QT[:, c, :], qs, 128.0 ** -0.5, eq, ALU.mult, ALU.mult, [qsk, eqk], [("QT", c)])
                elif kind == "c_q":
                    c = idx
                    if True:
                        gbank, gbk = mmbank()
                        mmg(gbank[:, :], [(wgkb[:, l * 256 + c * 128:l * 256 + (c + 1) * 128], crT[:, :])],
                            [("wgkb",), ("crT",)], [gbk])
                        ee, eek = scratch()
                        act(ee, gbank[:, :], AF.Exp, [gbk, ("negbgk",)], [eek], scale=-1.0,
                            bias=negbgk[:, l * 2 + c:l * 2 + c + 1])
                        sp, spk = scratch()
                        ts("dve", ee, ee, 1.0, None, ALU.add, None, [eek], [eek])
                        act(sp, ee, AF.Ln, [eek], [spk])
                        Bt, Bk = scratch()
                        P.op("dve", lambda e, Bt=Bt, sp=sp: e.tensor_tensor_scan(
                            out=Bt, data0=cc("cmask", 0, 512), data1=sp, initial=0.0, op0=ALU.mult, op1=ALU.add),
                            [spk, CST], [Bk])
                        eq, eqk, ek, ekk = gate_generic(Bt, Bk, 6 + c, -1.0 / 16.0)
                        state["eqC"] = (eq, eqk, ek, ekk)
                    eq, eqk, ek, ekk = state["eqC"]
                    qf, qfk = scratch()
                    cp("act", qf, bank[:, :], [bk], [qfk])
                    for hh_ in range(2):
                        stt("dve", QT[:, 8 + 2 * c + hh_, :], qf, cc("hm8", hh_, 1), eq, ALU.mult, ALU.mult,
                            [qfk, eqk, CST], [("QT", 8 + 2 * c + hh_)])
                elif kind == "c_k":
                    c = idx
                    eq, eqk, ek, ekk = state["eqC"]
                    kf, kfk = scratch()
                    cp("act", kf, bank[:, :], [bk], [kfk])
                    tt("pool", KT[:, 6 + c, :], kf, ek, ALU.mult, [kfk, ekk], [("KT", 6 + c)])
                    k2_flush(2)
                    j = nxt("k2t", 3)
                    tt("pool", K2T[:, j, :].rearrange("p (c t) -> p c t", t=64),
                       KT[:, 6 + c, :].rearrange("p (c t) -> p c t", t=64),
                       eq.rearrange("p (c t) -> p c t", t=64)[:, :, 63:64].to_broadcast([128, 8, 64]), ALU.mult,
                       [("KT", 6 + c), eqk], [("K2T", j)])
                    k2_transposes(K2T[:, j, :], ("K2T", j), 6 + c, chm_cols)
                elif kind in ("b_q", "b_k"):
                    t1, t1k = scratch()
                    tt("dve", t1, bank[:, :], cosb[:], ALU.mult, [bk, ("cos",)], [t1k])
                    state["t1"] = (t1, t1k)
                elif kind in ("b_qs", "b_ks"):
                    c = idx
                    t1, t1k = state["t1"]
                    t2, t2k = scratch()
                    tt("dve", t2, bank[:, :], sinb[:], ALU.mult, [bk, ("sin",)], [t2k])
                    tt("pool", t2, t2, t1, ALU.add, [t2k, t1k], [t2k])
                    t23 = t2.rearrange("p (c t) -> p c t", t=64)
                    if kind == "b_qs":
                        for hh_ in range(2):
                            h = 2 * c + hh_
                            tab = cst[:, COFF["eqBz"] + h * 64:COFF["eqBz"] + (h + 1) * 64]
                            tt("pool" if hh_ else "dve", QT[:, 4 + h, :].rearrange("p (c t) -> p c t", t=64), t23,
                               tab.unsqueeze(1).to_broadcast([128, 8, 64]), ALU.mult, [t2k, CST], [("QT", 4 + h)])
                    else:
                        tab = cst[:, COFF["ekB"] + c * 64:COFF["ekB"] + (c + 1) * 64]
                        tt("dve", KT[:, 4 + c, :].rearrange("p (c t) -> p c t", t=64), t23,
                           tab.unsqueeze(1).to_broadcast([128, 8, 64]), ALU.mult, [t2k, CST], [("KT", 4 + c)])
                        k2_flush(2)
                        j = nxt("k2t", 3)
                        ts("dve", K2T[:, j, :], KT[:, 4 + c, :], c2b[:, c:c + 1], None, ALU.mult, None,
                           [("KT", 4 + c), ("c2b",)], [("K2T", j)])
                        k2_transposes(K2T[:, j, :], ("K2T", j), 4 + c, chm_cols)
                elif kind == "mg":
                    act(big[:, idx * 512:(idx + 1) * 512], bank[:, :], AF.Sigmoid, [bk], [("big", idx)])

            k2_flush(0)
            for t_ in range(6):
                ri = use_slot(l, S_TOK + t_)
                for s in range(NSUB):
                    bank, bk = mmbank()
                    mmg(bank[:, :], [(xn[:, kc, s * 128:(s + 1) * 128], ring[:, ri, kc * 512:(kc + 1) * 512])
                                     for kc in range(8)], XN_ALL + [("wr", ri)], [bk])
                    if t_ % 2 == 0:
                        cp("act", tokv[:, s, t_ * 512:(t_ + 1) * 512], bank[:, :], [bk], [("tokv", s, t_)])
                    else:
                        act(tokv[:, s, t_ * 512:(t_ + 1) * 512], bank[:, :], AF.Silu, [bk], [("tokv", s, t_)])

            MIX = [
                ("A", 0, 4), ("B", 4, 2), ("C", 6, 2)]
            y_pend = []
            n_pend = []
            n2_pend = []
            for s in range(NSUB):
                par = s % 2
                for mi, (mname, b0, nb) in enumerate(MIX):
                    vseg = 2 * mi
                    SK = ("S", l, mi)

                    def qidx(h):
                        return h if mi == 0 else (4 + h if mi == 1 else 8 + h)

                    def kblk(h):
                        return h if mi == 0 else b0 + h // 2

                    items = []
                    rd = [("mask4",)]
                    for h in range(4):
                        items.append(("mm", p_sc[:, h * 128:(h + 1) * 128], KT[:, kblk(h), s * 128:(s + 1) * 128],
                                      QT[:, qidx(h), s * 128:(s + 1) * 128], True, True))
                        rd += [("KT", kblk(h)), ("QT", qidx(h))]
                    pe_multi(items, rd, [("sc",)])
                    si = nxt("scm", 2)
                    tt("dve", scm[:, si, :], p_sc[:, :], mask4[:].rearrange("p h i -> p (h i)"), ALU.mult,
                       [("sc",), ("mask4",)], [("scm", si)])
                    for ch in range(2):
                        ci_ = s * 2 + ch
                        erk = [("eref", b0 + j) for j in range(nb)]
                        elk = [("elast", b0 + j) for j in range(nb)]
                        tt("pool", sref[:, ch, b0:b0 + nb, :], S[:, l, b0:b0 + nb, :],
                           eref[:, b0:b0 + nb, ci_:ci_ + 1].to_broadcast([128, nb, 128]), ALU.mult,
                           [SK] + erk, [("sref", ch, mi)])
                        items = []
                        rd = [("tokv", s, vseg)]
                        for h in range(4):
                            if mi == 0:
                                o_ = p_kv[:, h * 128:(h + 1) * 128]
                                l_ = K2tok[:, ch, s, h * 128:(h + 1) * 128]
                            else:
                                p0 = 64 * (h % 2)
                                blk = b0 + h // 2
                                o_ = p_kv[p0:p0 + 64, (h // 2) * 128:(h // 2 + 1) * 128]
                                l_ = K2tok[:, ch, s, blk * 128 + p0:blk * 128 + p0 + 64]
                            items.append(("mm", o_, l_, tokv[:, s, vseg * 512 + h * 128:vseg * 512 + (h + 1) * 128],
                                          True, True))
                            rd.append(("K2tok", ch, kblk(h)))
                        pe_multi(items, rd, [("kv",)])
                        tt("pool", S[:, l, b0:b0 + nb, :], S[:, l, b0:b0 + nb, :],
                           elast[:, b0:b0 + nb, ci_:ci_ + 1].to_broadcast([128, nb, 128]), ALU.mult,
                           [SK] + elk, [SK])
                        tt("dve", S[:, l, b0:b0 + nb, :], S[:, l, b0:b0 + nb, :],
                           p_kv[:, 0:nb * 128].rearrange("p (b v) -> p b v", b=nb), ALU.add, [SK, ("kv",)], [SK])
                    y_flush_after_o = list(y_pend)
                    del y_pend[:]
                    oi = nxt("o", 3)
                    po = O_BANKS[oi]
                    okey = O_KEYS[oi]
                    items = []
                    rd = [("scm", si), ("tokv", s, vseg), ("sref", 0, mi), ("sref", 1, mi)]
                    for h in range(4):
                        oc = slice(h * 128, (h + 1) * 128)
                        items.append(("mm", po[:, oc], scm[:, si, oc],
                                      tokv[:, s, vseg * 512 + h * 128:vseg * 512 + (h + 1) * 128], True, False))
                        for ch in range(2):
                            items.append(("mm", po[ch * 64:(ch + 1) * 64, oc],
                                          QT[:, qidx(h), s * 128 + ch * 64:s * 128 + (ch + 1) * 64],
                                          sref[:, ch, kblk(h), :], False, ch == 1))
                        rd.append(("QT", qidx(h)))
                    pe_multi(items, rd, [okey])
                    for f_ in y_flush_after_o:
                        f_()
                    def norm_p1(s=s, par=par, mi=mi, vseg=vseg, okey=okey, po=po):
                        po3 = po[:, :].rearrange("p (h v) -> p h v", h=4)
                        sq, sqk = scratch()
                        act(sq, po[:, :], AF.Square, [okey], [sqk])
                        st, stk = stat()
                        P.op("dve", lambda e, st=st, sq=sq: e.tensor_reduce(
                            out=st[:, 0:4], in_=sq.rearrange("p (h v) -> p h v", h=4), axis=AX.X, op=ALU.add),
                            [sqk], [stk])
                        st2, st2k = None, None
                        if mi != 1:
                            act(st[:, 0:4], st[:, 0:4], AF.Ln, [stk, CST], [stk], scale=1.0 / 128.0, bias=cc("eps", 0, 1))
                            act(st[:, 0:4], st[:, 0:4], AF.Exp, [stk], [stk], scale=-0.5)
                        else:
                            P.op("dve", lambda e, st=st, po3=po3: e.tensor_reduce(
                                out=st[:, 4:8], in_=po3, axis=AX.X, op=ALU.add), [okey, stk], [stk])
                            st2, st2k = stat()
                            ts("dve", st2[:, 0:4], st[:, 4:8], 1.0 / 128.0, None, ALU.mult, None, [stk], [st2k])
                            tt("dve", st2[:, 4:8], st2[:, 0:4], st2[:, 0:4], ALU.mult, [st2k], [st2k])
                            stt("dve", st[:, 0:4], st[:, 0:4], 1.0 / 128.0, st2[:, 4:8], ALU.mult, ALU.subtract,
                                [stk, st2k], [stk])
                            act(st[:, 0:4], st[:, 0:4], AF.Ln, [stk, CST], [stk], bias=cc("eps", 0, 1))
                            act(st[:, 0:4], st[:, 0:4], AF.Exp, [stk], [stk], scale=-0.5)

                        def norm_p2(s=s, par=par, mi=mi, vseg=vseg, okey=okey, po3=po3, st=st, stk=stk,
                                    st2=st2, st2k=st2k):
                            yv = ybuf[:, par, mi * 512:(mi + 1) * 512]
                            yk = ("y", par, mi)
                            gseg = tokv[:, s, (vseg + 1) * 512:(vseg + 2) * 512]
                            yt, ytk = scratch()
                            yt3 = yt.rearrange("p (h v) -> p h v", h=4)
                            if mi != 1:
                                tt("dve", yt3, po3, st[:, 0:4].unsqueeze(2).to_broadcast([128, 4, 128]), ALU.mult,
                                   [okey, stk], [ytk])
                            else:
                                tt("dve", yt3, po3, st2[:, 0:4].unsqueeze(2).to_broadcast([128, 4, 128]),
                                   ALU.subtract, [okey, st2k], [ytk])
                                tt("pool", yt3, yt3, st[:, 0:4].unsqueeze(2).to_broadcast([128, 4, 128]), ALU.mult,
                                   [ytk, stk], [ytk])
                            tt("pool", yv, yt, gseg, ALU.mult, [ytk, ("tokv", s, vseg + 1)], [yk])

                            def ytr(par=par, mi=mi, s=s, yk=yk):
                                half = nxt("tr", 2)
                                items = []
                                for h in range(4):
                                    items.append(("tr", p_tr[:, half, h * 128:(h + 1) * 128],
                                                  ybuf[:, par, mi * 512 + h * 128:mi * 512 + (h + 1) * 128],
                                                  identb[:], True, True))
                                pe_multi(items, [yk, ("identb",)], [("tr",)])
                                src = p_tr[:, half, :].rearrange("p (h t) -> p h t", h=4)
                                dst = yT[:, mi * 4:(mi + 1) * 4, s * 128:(s + 1) * 128]
                                if mi == 0:
                                    act(dst, src, AF.Copy, [("tr",), CST], [("yT", mi, s)], scale=cc("gna", l, 1))
                                elif mi == 2:
                                    act(dst, src, AF.Copy, [("tr",), CST], [("yT", mi, s)], scale=cc("gnc", l, 1))
                                else:
                                    cp("act", dst, src, [("tr",)], [("yT", mi, s)])
                            y_pend.append(ytr)
                        n2_pend.append(norm_p2)
                    run2 = list(n2_pend)
                    del n2_pend[:]
                    for f_ in run2:
                        f_()
                    run1 = list(n_pend)
                    del n_pend[:]
                    for f_ in run1:
                        f_()
                    n_pend.append(norm_p1)
            for _ in range(4):
                for lst in (y_pend, n2_pend, n_pend):
                    run_ = list(lst)
                    del lst[:]
                    for f_ in run_:
                        f_()

            rbr = [use_slot(l, S_BR + br) for br in range(3)]
            for dc in range(8):
                accs = []
                for br in range(3):
                    bank, bk = mmbank()
                    mmg(bank[:, :], [(ring[:, rbr[br], (dc * 4 + kc) * 128:(dc * 4 + kc + 1) * 128],
                                      yT[:, br * 4 + kc, :]) for kc in range(4)],
                        [("wr", rbr[br])] + [("yT", br, s) for s in range(NSUB)], [bk])
                    a, ak = scratch()
                    g = br * 8 + dc
                    tt("dve", a, bank[:, :], big[:, g * 512:(g + 1) * 512], ALU.mult, [bk, ("big", g)], [ak])
                    accs.append((a, ak))
                tt("pool", accs[0][0], accs[0][0], accs[1][0], ALU.add, [accs[0][1], accs[1][1]], [accs[0][1]])
                tt("pool", xn[:, dc, :], accs[0][0], accs[2][0], ALU.add, [accs[0][1], accs[2][1]], [("xn", dc)])
            for i in range(2):
                ri = use_slot(l, S_OUT + i)
                for dl in range(4):
                    dc = 4 * i + dl
                    bank, bk = mmbank()
                    mmg(bank[:, :], [(ring[:, ri, (dl * 8 + kc) * 128:(dl * 8 + kc + 1) * 128], xn[:, kc, :])
                                     for kc in range(8)], XN_ALL + [("wr", ri)], [bk])
                    tt("dve", xT[:, dc, :], xT[:, dc, :], bank[:, :], ALU.add, [("xT", dc), bk], [("xT", dc)])

            rmsnorm_to_xn("nffn", l)
            for i in range(11):
                ri = use_slot(l, S_FFN + i)
                for j in range(2):
                    fc = 2 * i + j
                    bg, bgk_ = mmbank()
                    mmg(bg[:, :], [(ring[:, ri, (2 * j * 8 + kc) * 128:(2 * j * 8 + kc + 1) * 128], xn[:, kc, :])
                                   for kc in range(8)], XN_ALL + [("wr", ri)], [bgk_])
                    bu, buk = mmbank()
                    mmg(bu[:, :], [(ring[:, ri, ((2 * j + 1) * 8 + kc) * 128:((2 * j + 1) * 8 + kc + 1) * 128],
                                    xn[:, kc, :]) for kc in range(8)], XN_ALL + [("wr", ri)], [buk])
                    sg, sgk = scratch()
                    act(sg, bg[:, :], AF.Silu, [bgk_], [sgk])
                    tt("dve", big[:, fc * 512:(fc + 1) * 512], sg, bu[:, :], ALU.mult, [sgk, buk], [("big", fc)])
            for dc in range(8):
                ri = use_slot(l, S_DOWN + dc)
                bank, bk = mmbank()
                mmg(bank[:, :], [(ring[:, ri, kc * 128:(kc + 1) * 128], big[:, kc * 512:(kc + 1) * 512])
                                 for kc in range(FC)], [("big", kc) for kc in range(FC)] + [("wr", ri)], [bk])
                tt("dve", xT[:, dc, :], xT[:, dc, :], bank[:, :], ALU.add, [("xT", dc), bk], [("xT", dc)])

        identf = cc("ident", 0, 128)
        xio = big[:, 0:2 * SLOTW].bitcast(F32).rearrange("p (s d) -> p s d", s=NSUB)
        xio_keys = [("big", u) for u in range(16)]
        xout = tokv[:].rearrange("p s c -> p (s c)")[:, 0:2 * SLOTW].bitcast(F32).rearrange("p (s d) -> p s d", s=NSUB)

        def xout_keys(s, hf):
            e0 = s * 2048 + hf * 1024
            return sorted({("tokv", e // 3072, (e % 3072) // 512) for e in range(e0, e0 + 1024, 512)})
        xout_all = sorted({k for s in range(NSUB) for hf in range(2) for k in xout_keys(s, hf)})
        for sq_ in range(n_seq):
            for l in range(depth):
                P.op("pool", lambda e, l=l: e.memset(S[:, l, :, :], 0.0), [], [("S", l, 0), ("S", l, 1), ("S", l, 2)])
            for tl in range(n_tiles):
                t0 = sq_ * seq_t + tl * T
                dma("sp", xio, x_d[t0:t0 + T, :].rearrange("(s p) d -> p s d", p=128), "xio", [], xio_keys)
                dma("sp", cosb[:], cos_d[:, tl * T:(tl + 1) * T], "cos", [], [("cos",)])
                dma("sp", sinb[:], sin_d[:, tl * T:(tl + 1) * T], "sin", [], [("sin",)])
                for dc in range(8):
                    bank, bk = mmbank()
                    items = [("tr", bank[:, s * 128:(s + 1) * 128], xio[:, s, dc * 128:(dc + 1) * 128], identf,
                              True, True) for s in range(NSUB)]
                    pe_multi(items, xio_keys + [CST], [bk])
                    cp("act" if dc % 2 else "dve", xT[:, dc, :], bank[:, :], [bk], [("xT", dc)])
                for l in range(depth):
                    layer(l, tl)
                for dc in range(8):
                    if dc < 5:
                        act(xn[:, dc, :], xT[:, dc, :], AF.Square, [("xT", dc)], [("xn", dc)])
                    else:
                        tt("pool", xn[:, dc, :], xT[:, dc, :], xT[:, dc, :], ALU.mult, [("xT", dc)], [("xn", dc)])
                bank, bk = mmbank()
                mmg(bank[:, :], [(onesb[:], xn[:, dc, :]) for dc in range(8)], XN_ALL + [("onesb",)], [bk])
                act(rstd[:], bank[:, :], AF.Ln, [bk, CST], [("rstd",)], scale=1.0 / D, bias=cc("eps", 0, 1))
                act(rstd[:], rstd[:], AF.Exp, [("rstd",)], [("rstd",)], scale=-0.5)
                for dc in range(8):
                    stt("dve", xT[:, dc, :], xT[:, dc, :], cc("nfin", dc, 1), rstd[:],
                        ALU.mult, ALU.mult, [("xT", dc), ("rstd",), CST], [("xT", dc)])
                for s in range(NSUB):
                    for hf in range(2):
                        bank, bk = mmbank()
                        items = [("tr", bank[:, j * 128:(j + 1) * 128], xT[:, hf * 4 + j, s * 128:(s + 1) * 128],
                                  identf, True, True) for j in range(4)]
                        pe_multi(items, [("xT", hf * 4 + j) for j in range(4)] + [CST], [bk])
                        cp("act" if hf else "dve", xout[:, s, hf * 512:(hf + 1) * 512], bank[:, :], [bk],
                           xout_keys(s, hf))
                dma("act", out_d[t0:t0 + T, :].rearrange("(s p) d -> p s d", p=128), xout, "xout", xout_all, [])

        P.finalize(getsem)
        block = es.enter_context(nc.Block())

        @block.tensor
        def _(e):
            P.emit_engine("pe", e)

        @block.scalar
        def _(e):
            P.emit_engine("act", e)

        @block.vector
        def _(e):
            P.emit_engine("dve", e)

        @block.gpsimd
        def _(e):
            P.emit_engine("pool", e)

        @block.sync
        def _(e):
            P.emit_engine("sp", e, final_waits=True)
    return nc


def run(inputs, n_seq, n_tiles, depth):
    import time
    t0 = time.time()
    x = np.ascontiguousarray(inputs["x"], dtype=np.float32)
    seq_t = n_tiles * T
    cst, extra = build_consts(inputs)
    wsrc = build_wsrc(inputs, depth)
    cosT, sinT = rope_tables(seq_t)
    print("[kernel] host prep %.1fs" % (time.time() - t0), flush=True)
    t0 = time.time()
    nc = build_program(n_seq, n_tiles, depth)
    print("[kernel] build %.1fs" % (time.time() - t0), flush=True)
    t0 = time.time()
    in_maps = []
    for c in range(NCORES):
        xc = x[c * n_seq:(c + 1) * n_seq].reshape(n_seq * seq_t, D)
        in_maps.append({"x": xc, "wsrc": wsrc, "cst": cst, "c2b": extra["c2B"], "wgk": extra["wgk"], "cosT": cosT, "sinT": sinT})
    ncr = _TEST_CORES or NCORES
    res = run_bass_kernel_spmd(nc, in_maps[:ncr], core_ids=list(range(ncr)))
    print("[kernel] compile+run %.1fs" % (time.time() - t0), flush=True)
    outs = [np.asarray(r["out"]).reshape(n_seq, seq_t, D) for r in res.results]
    while len(outs) < NCORES:
        outs.append(np.zeros_like(outs[0]))
    return np.concatenate(outs, axis=0).astype(np.float32)


def kernel(**inputs):
    return run(inputs, BATCH // NCORES, SEQ // T, DEPTH)
```
